# Optimizing a Trainium2 kernel written in Bass

```python
import jax, jax.numpy as jnp
from jax import lax
import numpy as np

D_MODEL = 1024
BATCH = 8
SEQ = 2048
DEPTH = 2

D_CONV = D_MODEL // 2
D_POOL = D_MODEL // 2
D_SC = D_MODEL // 2
CONV_A_K = 31
SC_K = 3
POOL_WINDOWS = (2, 4, 8, 16)
N_POOL_GROUPS = len(POOL_WINDOWS)
POOL_GROUP_DIM = D_POOL // N_POOL_GROUPS
POOL_OUT_DIM = D_MODEL // N_POOL_GROUPS
MAX_WINDOW = max(POOL_WINDOWS)
N_BRANCH = 3
PROJ_SPLITS = (D_CONV, 2 * D_CONV, 2 * D_CONV + D_POOL, 2 * D_CONV + D_POOL + D_SC,
               2 * D_CONV + D_POOL + 2 * D_SC, 2 * D_CONV + D_POOL + 3 * D_SC)
D_IN_PROJ = 2 * D_CONV + D_POOL + 3 * D_SC + N_BRANCH * D_MODEL
N_MEM = 256
XATTN_HEADS = 4
XATTN_HEAD_DIM = D_MODEL // XATTN_HEADS
MOE_GROUPS = 4
EXPERTS_PER_GROUP = 4
N_EXPERTS = MOE_GROUPS * EXPERTS_PER_GROUP
EXPERT_TOP_K = 2
D_EXPERT = D_MODEL // 4
EPS = 1e-6

kernel_name = "hybrid_gated_conv_pool_shortconv_hiermoe"


def rmsnorm(x, g):
    xf = x.astype(jnp.float32)
    y = xf * lax.rsqrt(jnp.mean(xf * xf, axis=-1, keepdims=True) + EPS)
    return (y * g.astype(jnp.float32)).astype(x.dtype)


def layernorm(x, g, b):
    xf = x.astype(jnp.float32)
    mu = jnp.mean(xf, axis=-1, keepdims=True)
    var = jnp.mean(jnp.square(xf - mu), axis=-1, keepdims=True)
    y = (xf - mu) * lax.rsqrt(var + EPS)
    return (y * g.astype(jnp.float32) + b.astype(jnp.float32)).astype(x.dtype)


def causal_dwconv(u, w):
    k = w.shape[0]
    u_pad = jnp.pad(u, ((0, 0), (k - 1, 0), (0, 0)))
    return lax.conv_general_dilated(
        u_pad, w[:, None, :].astype(u.dtype), window_strides=(1,), padding='VALID',
        dimension_numbers=('NWC', 'WIO', 'NWC'), feature_group_count=u.shape[-1])


def multiscale_pool_minus_token(u):
    s = u.shape[1]
    uf = u.astype(jnp.float32)
    cs = jnp.cumsum(uf, axis=1)
    cs_pad = jnp.pad(cs, ((0, 0), (MAX_WINDOW, 0), (0, 0)))
    t = jnp.arange(s, dtype=jnp.int32)
    outs = []
    for i, w in enumerate(POOL_WINDOWS):
        c0, c1 = i * POOL_GROUP_DIM, (i + 1) * POOL_GROUP_DIM
        prev = cs_pad[:, MAX_WINDOW - w:MAX_WINDOW - w + s, c0:c1]
        cnt = jnp.minimum(t + 1, w).astype(jnp.float32)[None, :, None]
        outs.append((cs[:, :, c0:c1] - prev) / cnt - uf[:, :, c0:c1])
    return jnp.concatenate(outs, axis=-1).astype(u.dtype)


def gated_branch_mixer(h, w_in, conv_a_w, conv_a_b, ln_a_g, ln_a_b, w_a_out,
                       w_pool_grp, pool_scale, conv_c_w, w_c_out, w_o):
    bsz, s, _ = h.shape
    proj = h @ w_in
    a_val, a_gate, u_pool, c_x, c_b, c_c, g = jnp.split(proj, PROJ_SPLITS, axis=-1)
    a = a_val * jax.nn.sigmoid(a_gate)
    a = causal_dwconv(a, conv_a_w) + conv_a_b
    a = jax.nn.silu(layernorm(a, ln_a_g, ln_a_b)) @ w_a_out
    p = multiscale_pool_minus_token(u_pool).reshape(bsz, s, N_POOL_GROUPS, POOL_GROUP_DIM)
    p = jnp.einsum('bsgc,gco->bsgo', p, w_pool_grp).reshape(bsz, s, D_MODEL) * pool_scale
    c = (c_b * causal_dwconv(c_c * c_x, conv_c_w)) @ w_c_out
    gates = jax.nn.sigmoid(g).reshape(bsz, s, N_BRANCH, D_MODEL)
    merged = gates[:, :, 0] * a + gates[:, :, 1] * p + gates[:, :, 2] * c
    return merged @ w_o


def memory_cross_attention(h, mem_n, w_xq, w_xkv, w_xo):
    bsz, s, _ = h.shape
    q = (h @ w_xq).reshape(bsz, s, XATTN_HEADS, XATTN_HEAD_DIM)
    k, v = jnp.split(mem_n @ w_xkv, 2, axis=-1)
    k = k.reshape(bsz, N_MEM, XATTN_HEADS, XATTN_HEAD_DIM)
    v = v.reshape(bsz, N_MEM, XATTN_HEADS, XATTN_HEAD_DIM)
    scores = jnp.einsum('bshd,bmhd->bhsm', q, k).astype(jnp.float32) * (XATTN_HEAD_DIM ** -0.5)
    probs = jax.nn.softmax(scores, axis=-1).astype(v.dtype)
    o = jnp.einsum('bhsm,bmhd->bshd', probs, v).reshape(bsz, s, D_MODEL)
    return o @ w_xo


def hierarchical_moe(h, w_rg, b_rg, w_re, b_re, w_e_gate, w_e_up, w_e_down):
    bsz, s, d = h.shape
    t = h.reshape(-1, d)
    n = t.shape[0]
    p_grp = jax.nn.softmax((t @ w_rg + b_rg).astype(jnp.float32), axis=-1)
    g_val, g_idx = lax.top_k(p_grp, 1)
    le = (t @ w_re + b_re).astype(jnp.float32).reshape(n, MOE_GROUPS, EXPERTS_PER_GROUP)
    le_sel = jnp.take_along_axis(le, g_idx[:, :, None], axis=1)[:, 0]
    p_exp = jax.nn.softmax(le_sel, axis=-1)
    e_val, e_idx = lax.top_k(p_exp, EXPERT_TOP_K)
    e_val = e_val / jnp.sum(e_val, axis=-1, keepdims=True)
    weight = g_val * e_val
    eid = g_idx * EXPERTS_PER_GROUP + e_idx
    gate = jnp.sum(jax.nn.one_hot(eid, N_EXPERTS, dtype=jnp.float32) * weight[..., None], axis=1)
    act = jax.nn.silu(jnp.einsum('nd,edf->nef', t, w_e_gate)) * jnp.einsum('nd,edf->nef', t, w_e_up)
    act = act * gate[:, :, None].astype(act.dtype)
    out = jnp.einsum('nef,efd->nd', act, w_e_down)
    return out.reshape(bsz, s, d)


def setup_inputs(seed: int = 0) -> dict:
    key = jax.random.key(seed)
    ks = jax.random.split(key, 32)
    f32 = jnp.float32

    def nrm(k, shape, scale):
        return jax.random.normal(k, shape, f32) * scale

    def gain(k, shape):
        return 1.0 + 0.02 * jax.random.normal(k, shape, f32)

    L = DEPTH
    return {
        "x": nrm(ks[0], (BATCH, SEQ, D_MODEL), 1.0),
        "mem": nrm(ks[1], (BATCH, N_MEM, D_MODEL), 1.0),
        "norm_mix_g": gain(ks[2], (L, D_MODEL)),
        "w_in": nrm(ks[3], (L, D_MODEL, D_IN_PROJ), D_MODEL ** -0.5),
        "conv_a_w": nrm(ks[4], (L, CONV_A_K, D_CONV), CONV_A_K ** -0.5),
        "conv_a_b": nrm(ks[5], (L, D_CONV), 0.02),
        "ln_a_g": gain(ks[6], (L, D_CONV)),
        "ln_a_b": nrm(ks[7], (L, D_CONV), 0.02),
        "w_a_out": nrm(ks[8], (L, D_CONV, D_MODEL), D_CONV ** -0.5),
        "w_pool_grp": nrm(ks[9], (L, N_POOL_GROUPS, POOL_GROUP_DIM, POOL_OUT_DIM), POOL_GROUP_DIM ** -0.5),
        "pool_scale": gain(ks[10], (L, D_MODEL)),
        "conv_c_w": nrm(ks[11], (L, SC_K, D_SC), SC_K ** -0.5),
        "w_c_out": nrm(ks[12], (L, D_SC, D_MODEL), D_SC ** -0.5),
        "w_o": nrm(ks[13], (L, D_MODEL, D_MODEL), D_MODEL ** -0.5),
        "norm_x_g": gain(ks[14], (L, D_MODEL)),
        "norm_mem_g": gain(ks[15], (L, D_MODEL)),
        "w_xq": nrm(ks[16], (L, D_MODEL, D_MODEL), D_MODEL ** -0.5),
        "w_xkv": nrm(ks[17], (L, D_MODEL, 2 * D_MODEL), D_MODEL ** -0.5),
        "w_xo": nrm(ks[18], (L, D_MODEL, D_MODEL), D_MODEL ** -0.5),
        "norm_ffn_g": gain(ks[19], (L, D_MODEL)),
        "w_rg": nrm(ks[20], (L, D_MODEL, MOE_GROUPS), D_MODEL ** -0.5),
        "b_rg": nrm(ks[21], (L, MOE_GROUPS), 0.01),
        "w_re": nrm(ks[22], (L, D_MODEL, N_EXPERTS), D_MODEL ** -0.5),
        "b_re": nrm(ks[23], (L, N_EXPERTS), 0.01),
        "w_e_gate": nrm(ks[24], (L, N_EXPERTS, D_MODEL, D_EXPERT), D_MODEL ** -0.5),
        "w_e_up": nrm(ks[25], (L, N_EXPERTS, D_MODEL, D_EXPERT), D_MODEL ** -0.5),
        "w_e_down": nrm(ks[26], (L, N_EXPERTS, D_EXPERT, D_MODEL), D_EXPERT ** -0.5),
        "norm_f_g": gain(ks[27], (D_MODEL,)),
    }


def reference(x, mem, norm_mix_g, w_in, conv_a_w, conv_a_b, ln_a_g, ln_a_b, w_a_out,
              w_pool_grp, pool_scale, conv_c_w, w_c_out, w_o, norm_x_g, norm_mem_g,
              w_xq, w_xkv, w_xo, norm_ffn_g, w_rg, b_rg, w_re, b_re,
              w_e_gate, w_e_up, w_e_down, norm_f_g):
    for l in range(DEPTH):
        h = rmsnorm(x, norm_mix_g[l])
        x = x + gated_branch_mixer(h, w_in[l], conv_a_w[l], conv_a_b[l], ln_a_g[l], ln_a_b[l],
                                   w_a_out[l], w_pool_grp[l], pool_scale[l], conv_c_w[l],
                                   w_c_out[l], w_o[l])
        x = x + memory_cross_attention(rmsnorm(x, norm_x_g[l]), rmsnorm(mem, norm_mem_g[l]),
                                       w_xq[l], w_xkv[l], w_xo[l])
        x = x + hierarchical_moe(rmsnorm(x, norm_ffn_g[l]), w_rg[l], b_rg[l], w_re[l], b_re[l],
                                 w_e_gate[l], w_e_up[l], w_e_down[l])
    return rmsnorm(x, norm_f_g)
```

```python
import numpy as np
import concourse.bass as bass
import concourse.mybir as mybir
from concourse.bass_utils import run_bass_kernel_spmd

F32 = mybir.dt.float32
BF16 = mybir.dt.bfloat16
AF = mybir.ActivationFunctionType
ALU = mybir.AluOpType

L = 2
D = 1024
T = 2048
NT = 4
TW = 512
NMEM = 256
DIN = 6144
NE = 16
EPS = 1e-6
NPL = 208
NPV = 2 * NPL + 29
NU = 7
NSLOT = NU * 512
HW = 1040
I32 = mybir.dt.int32
ENGS = ("pe", "act", "dve", "pool", "sp")


class Tile:
    __slots__ = ("w", "r", "name")

    def __init__(self, name="", base=None):
        self.name = name
        self.w = dict(base.w) if base is not None else {}
        self.r = dict(base.r) if base is not None else {}


def merged_base(tiles):
    b = Tile("base")
    for t in tiles:
        for k, v in t.w.items():
            if b.w.get(k, -1) < v:
                b.w[k] = v
        for k, v in t.r.items():
            if b.r.get(k, -1) < v:
                b.r[k] = v
    for k, v in b.r.items():
        if b.w.get(k, -1) < v:
            b.w[k] = v
    return b


class Op:
    __slots__ = ("fn", "waits", "dma", "idx")


class Planner:
    def __init__(self, nc):
        self.nc = nc
        self.ops = {e: [] for e in ENGS}
        self.seen = {e: {} for e in ENGS}
        self.clock = {}
        self.dcum = {}
        self.eng = {"pe": nc.tensor, "act": nc.scalar, "dve": nc.vector, "pool": nc.gpsimd, "sp": nc.sync}

    def op(self, eng, fn, reads=(), writes=(), dma=None, after=()):
        own = ("eng", eng)
        deps = {}
        for t in after:
            for k, v in t.w.items():
                if deps.get(k, -1) < v:
                    deps[k] = v
        for t in reads:
            for k, v in t.w.items():
                if deps.get(k, -1) < v:
                    deps[k] = v
        for t in writes:
            for k, v in t.w.items():
                if deps.get(k, -1) < v:
                    deps[k] = v
            for k, v in t.r.items():
                if deps.get(k, -1) < v:
                    deps[k] = v
        seen = self.seen[eng]
        waits = []
        for k, v in deps.items():
            if k == own and eng == "pe":
                continue
            if seen.get(k, -1) >= v:
                continue
            waits.append((k, v))
        for k, v in waits:
            for k2, v2 in self.clock[(k, v)].items():
                if seen.get(k2, -1) < v2:
                    seen[k2] = v2
            if seen.get(k, -1) < v:
                seen[k] = v
        o = Op()
        o.fn = fn
        o.waits = waits
        o.dma = dma
        o.idx = len(self.ops[eng])
        self.ops[eng].append(o)
        if dma is None:
            ev = (own, o.idx)
        else:
            self.dcum[dma] = self.dcum.get(dma, 0) + 16
            ev = (("dma", dma), self.dcum[dma])
        self.clock[ev] = dict(seen)
        for t in reads:
            if t.r.get(ev[0], -1) < ev[1]:
                t.r[ev[0]] = ev[1]
        for t in writes:
            t.w = {ev[0]: ev[1]}
            t.r = {}
        return ev

    def emit(self, final_waits):
        nc = self.nc
        need = {e: set() for e in ENGS}
        for e in ENGS:
            for o in self.ops[e]:
                for k, v in o.waits:
                    if k[0] == "eng":
                        need[k[1]].add(v)
        for k, v in final_waits:
            if k[0] == "eng":
                need[k[1]].add(v)
        EPOCH = 1000
        rank = {e: {idx: i for i, idx in enumerate(sorted(need[e]))} for e in ENGS}
        esem = {e: [nc.alloc_semaphore("es_%s_%d" % (e, j)) for j in range(len(need[e]) // EPOCH + 1)] for e in ENGS}
        dsem = {d: nc.alloc_semaphore("ds_" + d) for d in self.dcum}

        def dowait(engobj, k, v):
            if k[0] == "eng":
                r = rank[k[1]][v]
                engobj.wait_ge(esem[k[1]][r // EPOCH], r % EPOCH + 1)
            else:
                engobj.wait_ge(dsem[k[1]], v)

        for e in ENGS:
            engobj = self.eng[e]
            for o in self.ops[e]:
                for k, v in o.waits:
                    dowait(engobj, k, v)
                ins = o.fn()
                if o.dma is not None:
                    ins.then_inc(dsem[o.dma], 16)
                elif o.idx in need[e]:
                    ins.then_inc(esem[e][rank[e][o.idx] // EPOCH], 1)
        for k, v in final_waits:
            dowait(nc.sync, k, v)
        self.stats = {e: len(self.ops[e]) for e in ENGS}


def build(stage=None, n_layers=L, layers=None, skip=()):
    nc = bass.Bass("TRN2", target_bir_lowering=False)
    dt = nc.dram_tensor
    xT = dt("xT", [D, T], F32, kind="ExternalInput").ap()
    memT = dt("memT", [D, NMEM], F32, kind="ExternalInput").ap()
    pv = dt("pv", [128, NPV], F32, kind="ExternalInput").ap()
    cst = dt("cst", [128, 128 + 2048 + 128], F32, kind="ExternalInput").ap()
    w_in = dt("w_in", [L, D, DIN], F32, kind="ExternalInput").ap()
    w_a_out = dt("w_a_out", [L, 512, D], F32, kind="ExternalInput").ap()
    w_pool = dt("w_pool", [L, 4, 128, 256], F32, kind="ExternalInput").ap()
    w_c_out = dt("w_c_out", [L, 512, D], F32, kind="ExternalInput").ap()
    w_o = dt("w_o", [L, D, D], F32, kind="ExternalInput").ap()
    w_xq = dt("w_xq", [L, D, D], F32, kind="ExternalInput").ap()
    w_xkv = dt("w_xkv", [L, D, 2 * D], F32, kind="ExternalInput").ap()
    w_xo = dt("w_xo", [L, D, D], F32, kind="ExternalInput").ap()
    w_r = dt("w_r", [L, D, 20], F32, kind="ExternalInput").ap()
    w_eg = dt("w_eg", [L, NE, D, 256], F32, kind="ExternalInput").ap()
    w_eu = dt("w_eu", [L, NE, D, 256], F32, kind="ExternalInput").ap()
    w_ed = dt("w_ed", [L, NE, 128, 2 * D], F32, kind="ExternalInput").ap()
    outT = dt("outT", [D, T], F32, kind="ExternalOutput").ap()
    hs_all = dt("hs", [L * NSLOT, HW], BF16, kind="Internal").ap()
    wbf_g = dt("wbf_g", [L * NE * 128, 2048], BF16, kind="Internal").ap()
    wbf_u = dt("wbf_u", [L * NE * 128, 2048], BF16, kind="Internal").ap()
    wbf_d = dt("wbf_d", [L * NE * 128, 2048], BF16, kind="Internal").ap()
    ys_d = dt("ys", [NSLOT, D], F32, kind="Internal").ap()

    P = Planner(nc)
    op = P.op

    def sb(name, nf32):
        return nc.alloc_sbuf_tensor(name, [128, nf32], F32)[:]

    RX = sb("RX", 16384)
    RH = sb("RH", 8192)
    RMG = sb("RMG", 8192)
    RBR = sb("RBR", 4096)
    RW = [sb("RW%d" % i, 2048) for i in range(4)]
    RS = sb("RS", 4160)
    PV = sb("PV", NPV)
    RSQ = sb("RSQ", 512)
    RRS = sb("RRS", 1024)
    RSG = sb("RSG", 1024)
    RC = sb("RC", 128)

    PSB = [nc.alloc_psum_tensor("ps%d" % i, [128, 512], F32)[:] for i in range(8)]
    PSt = [Tile("ps%d" % i) for i in range(8)]
    psctr = [0]

    def ps():
        i = psctr[0] % 8
        psctr[0] += 1
        return PSt[i], PSB[i]

    def bfv(ap):
        return ap.bitcast(BF16)

    def tsl(tt):
        return slice(tt * TW, (tt + 1) * TW)

    X = [RX[:, c * T:(c + 1) * T] for c in range(8)]
    Xt = [[Tile("x%d_%d" % (c, t)) for t in range(NT)] for c in range(8)]
    HB = bfv(RH)
    H = [HB[:, c * T:(c + 1) * T] for c in range(8)]
    MGB = bfv(RMG)
    MG = [MGB[:, c * T:(c + 1) * T] for c in range(8)]
    ACONV = [RMG[:, c * T:(c + 1) * T] for c in range(4)]
    BRB = bfv(RBR)
    BR4 = [BRB[:, c * T:(c + 1) * T] for c in range(4)]
    SQ = [bfv(RSQ[:, i * 256:(i + 1) * 256]) for i in range(2)]
    SQt = [Tile("sq%d" % i) for i in range(2)]
    RSTD = [RRS[:, i * 512:(i + 1) * 512] for i in range(2)]
    RSTDt = [Tile("rstd%d" % i) for i in range(2)]
    SG = [RSG[:, i * 512:(i + 1) * 512] for i in range(2)]
    SGt = [Tile("sg%d" % i) for i in range(2)]
    ONES = bfv(RC[:, 0:64])
    IDENT = bfv(RC[:, 64:128])
    Ct = Tile("consts")
    PVt = Tile("pv")
    ctr = {"sq": 0, "rs": 0, "sg": 0}

    def nxt(kind, n=2):
        i = ctr[kind] % n
        ctr[kind] += 1
        return i

    op("sp", lambda: nc.sync.dma_start(out=PV, in_=pv), writes=[PVt], dma="pv")
    for tt in range(NT):
        for c in range(8):
            ev_ = op("sp", (lambda c=c, tt=tt: nc.sync.dma_start(out=X[c][:, tsl(tt)], in_=xT[c * 128:(c + 1) * 128, tsl(tt)])),
                     writes=[Xt[c][tt]], dma="x%d" % tt)
        for c in range(8):
            Xt[c][tt].w = {ev_[0]: ev_[1]}
    op("dve", lambda: nc.vector.memset(ONES, 1.0), writes=[Ct])
    op("pool", lambda: nc.gpsimd.dma_start(out=IDENT, in_=cst[:, 0:128]), writes=[Ct], dma="cst")

    ZT = Tile("zsrc")
    ZSRC = bfv(RMG)[:, 0:HW]
    op("dve", lambda: nc.vector.memset(ZSRC, 0.0), writes=[ZT])
    HSt = [[Tile("hs%d_%d" % (l_, i)) for i in range(8)] for l_ in range(L)]
    for l_ in range(L):
        for a in range(NSLOT // 128):
            op("sp", (lambda l_=l_, a=a: nc.sync.dma_start(out=hs_all[l_ * NSLOT + a * 128:l_ * NSLOT + (a + 1) * 128, :], in_=ZSRC)),
               reads=[ZT], after=([HSt[l_ - 1][a % 2]] if l_ > 0 else []), writes=[HSt[l_][a % 2]], dma="z%d" % (a % 2))
    WZt = Tile("wz")
    for wb_ in (wbf_g, wbf_u, wbf_d):
        for a in range(NE, L * NE):
            for h_ in range(2):
                op("sp", (lambda wb_=wb_, a=a, h_=h_: nc.sync.dma_start(
                    out=wb_[a * 128:(a + 1) * 128, h_ * 1024:(h_ + 1) * 1024], in_=ZSRC[:, 0:1024])),
                   reads=[ZT], writes=[WZt], dma="wz")
    YSt = [Tile("ys%d" % i) for i in range(2)]
    UT = bfv(RS[:, 4052:4116])
    UTt = Tile("ut")
    op("pool", lambda: nc.gpsimd.dma_start(out=UT, in_=cst[:, 128 + 2048:128 + 2048 + 128]), writes=[UTt], dma="ut")

    Wt = [Tile("w%d" % i) for i in range(4)]
    wctr = [0]

    def wslot():
        i = wctr[0] % 4
        wctr[0] += 1
        return i

    def wview(i, k, n):
        return bfv(RW[i])[:, 0:k * n].rearrange("p (k n) -> p k n", k=k)

    PCt = [[] for _ in range(L)]
    bgq = []
    for l_ in range(L):
        for e in range(NE):
            r0 = (l_ * NE + e) * 128
            for dstT, srcv in ((wbf_g, w_eg[l_, e].rearrange("(p k) n -> p (k n)", k=8)),
                               (wbf_u, w_eu[l_, e].rearrange("(p k) n -> p (k n)", k=8)),
                               (wbf_d, w_ed[l_, e])):
                bgq.append((l_, dstT[r0:r0 + 128, :], srcv))

    def bg_issue(n, upto_layer=None, first_reads=()):
        rd = list(first_reads)
        while bgq and n > 0:
            if upto_layer is not None and bgq[0][0] > upto_layer:
                break
            l_, dst_, src_ = bgq.pop(0)
            t_ = Tile("pc")
            op("pool", (lambda dst_=dst_, src_=src_: nc.gpsimd.dma_start(out=dst_, in_=src_)),
               after=rd + ([WZt] if l_ > 0 else []), writes=[t_], dma="pc%d" % l_)
            rd = []
            PCt[l_].append(t_)
            n -= 1

    bg_rate = [0]

    def wload(i, dst, src):
        op("pool", lambda: nc.gpsimd.dma_start(out=dst, in_=src), writes=[Wt[i]], dma="w%d" % i)
        bg_issue(bg_rate[0])

    def load_kn(wap, c0, n, k=8, slot=None):
        i = wslot() if slot is None else slot
        v = wview(i, k, n)
        wload(i, v, wap.rearrange("(k p) n -> p k n", p=128)[:, :, c0:c0 + n])
        return i, v

    def mm(pst, psa, lhsT, rhs, start, stop, reads):
        op("pe", lambda: nc.tensor.matmul(psa, lhsT, rhs, start=start, stop=stop), reads=reads, writes=[pst])

    def pcol(col):
        return PV[:, col:col + 1]

    EPSC = pcol(2 * NPL + 8)

    def rms_stats(srcs, srct, width, tt_slices):
        res = []
        for (sl, tts) in tt_slices:
            pt, pa = ps()
            pa = pa[:, 0:width]
            for c in range(8):
                i = nxt("sq")
                sq = SQ[i][:, 0:width]
                op("act", (lambda sq=sq, c=c, sl=sl: nc.scalar.activation(sq, srcs[c][:, sl], AF.Square)),
                   reads=[srct[c][tts]], writes=[SQt[i]])
                mm(pt, pa, ONES, sq, c == 0, c == 7, [SQt[i], Ct])
            j = nxt("rs")
            rs = RSTD[j][:, 0:width]
            op("act", (lambda rs=rs, pa=pa: nc.scalar.activation(rs, pa, AF.Ln, bias=EPSC, scale=1.0 / D)),
               reads=[pt, PVt], writes=[RSTDt[j]])
            op("act", (lambda rs=rs: nc.scalar.activation(rs, rs, AF.Exp, scale=-0.5)),
               reads=[RSTDt[j]], writes=[RSTDt[j]])
            res.append((RSTDt[j], rs))
        return res

    def norm_tile(tt, gcol, Ht):
        (rt, rs), = rms_stats(X, Xt, TW, [(tsl(tt), tt)])
        for c in range(8):
            op("dve", (lambda c=c, tt=tt, rs=rs: nc.vector.scalar_tensor_tensor(
                out=H[c][:, tsl(tt)], in0=X[c][:, tsl(tt)], scalar=pcol(gcol + c), in1=rs,
                op0=ALU.mult, op1=ALU.mult)),
               reads=[Xt[c][tt], rt, PVt], writes=[Ht[c][tt]])

    def rmsnorm_to_H(gcol, Ht):
        for tt in range(NT):
            norm_tile(tt, gcol, Ht)

    def new_tiles(name, n, m, base):
        return [[Tile("%s%d_%d" % (name, c, t), base) for t in range(m)] for c in range(n)]

    def flat(tl):
        return [t for row in tl for t in row]

    hbase = Tile("hb")
    Ht_pre = None
    final = []
    oi = [0]
    mgbase = merged_base([ZT])
    brbase = Tile("brb")
    sbase = Tile("sb")

    OB = [RS[:, i * 512:(i + 1) * 512] for i in range(3)]
    OBt = [None, None, None]

    def final_tile(tt):
        (rt, rs), = rms_stats(X, Xt, TW, [(tsl(tt), tt)])
        for c in range(8):
            i = oi[0] % 3
            oi[0] += 1
            if OBt[i] is None:
                OBt[i] = Tile("ob%d" % i, obbase[0])
            op("dve", (lambda c=c, tt=tt, rs=rs, i=i: nc.vector.scalar_tensor_tensor(
                out=OB[i], in0=X[c][:, tsl(tt)], scalar=pcol(2 * NPL + c), in1=rs, op0=ALU.mult, op1=ALU.mult)),
               reads=[Xt[c][tt], rt, PVt], writes=[OBt[i]])
            ev = op("sp", (lambda c=c, tt=tt, i=i: nc.sync.dma_start(out=outT[c * 128:(c + 1) * 128, tsl(tt)], in_=OB[i])),
                    reads=[OBt[i]], dma="o%d" % i)
            final.append(ev)

    obbase = [None]
    pre_w = [None]
    for l in (layers if layers is not None else range(n_layers)):
        pb = l * NPL
        if Ht_pre is None:
            Ht = new_tiles("h", 8, NT, hbase)
            rmsnorm_to_H(pb + 0, Ht)
        else:
            Ht = Ht_pre

        ACt = new_tiles("aconv", 4, NT, mgbase)
        APAD = [bfv(RS[:, i * 1040:i * 1040 + 1039]) for i in range(2)]
        APt = [[Tile("apad%d_%d" % (i, t), sbase) for t in range(NT + 1)] for i in range(2)]
        DG = [BRB[:, i * 3968:(i + 1) * 3968].rearrange("p (k n) -> p k n", k=31) for i in range(2)]
        DGt = [[Tile("dg%d_%d" % (i, k), brbase) for k in range(31)] for i in range(2)]
        for i in range(2):
            op("dve", (lambda i=i: nc.vector.memset(APAD[i][:, 0:30], 0.0)), writes=[APt[i][0]])
        if pre_w[0] is not None:
            (sv, Wv), (sg_, Wg) = pre_w[0]
            pre_w[0] = None
        else:
            sv, Wv = load_kn(w_in[l], 0, 512)
            sg_, Wg = load_kn(w_in[l], 512, 512)

        DGc = {0: (DG[0], DGt[0], None), 1: (DG[1], DGt[1], None)}

        def glu(c):
            ap_i = c % 2
            wc = pb + 40 + c * 31
            if c >= 2:
                si = wslot()
                DGc[c] = (wview(si, 31, 128), [Tile("dgr%d_%d" % (c, k), Wt[si]) for k in range(31)], si)
            dgv, dgt, _ = DGc[c]
            for k in range(31):
                op("act", (lambda dgv=dgv, k=k, wc=wc: nc.scalar.activation(dgv[:, k, :], IDENT, AF.Copy, scale=pcol(wc + k))),
                   reads=[Ct, PVt], writes=[dgt[k]])
            for tt in range(NT):
                pvt, pva = ps()
                for k in range(8):
                    mm(pvt, pva, Wv[:, k, c * 128:(c + 1) * 128], H[k][:, tsl(tt)], k == 0, k == 7, [Wt[sv], Ht[k][tt]])
                pgt, pga = ps()
                for k in range(8):
                    mm(pgt, pga, Wg[:, k, c * 128:(c + 1) * 128], H[k][:, tsl(tt)], k == 0, k == 7, [Wt[sg_], Ht[k][tt]])
                j = nxt("sg")
                op("act", (lambda j=j, pga=pga: nc.scalar.activation(SG[j], pga, AF.Sigmoid)),
                   reads=[pgt], writes=[SGt[j]])
                op("dve", (lambda j=j, pva=pva, ap_i=ap_i, tt=tt: nc.vector.tensor_tensor(
                    APAD[ap_i][:, 30 + tt * TW:30 + (tt + 1) * TW], pva, SG[j], op=ALU.mult)),
                   reads=[pvt, SGt[j]], writes=[APt[ap_i][tt + 1]])

        def conv(c, tts=range(NT)):
            ap_i = c % 2
            dgv, dgt, _ = DGc[c]
            for tt in tts:
                pt, pa = ps()
                for k in range(31):
                    mm(pt, pa, dgv[:, k, :], APAD[ap_i][:, tt * TW + k:tt * TW + k + TW], k == 0, k == 30,
                       [dgt[k], APt[ap_i][tt], APt[ap_i][tt + 1]])
                op("act", (lambda c=c, tt=tt, pa=pa, pb=pb: nc.scalar.activation(
                    ACONV[c][:, tsl(tt)], pa, AF.Identity, bias=pcol(pb + 164 + c))),
                   reads=[pt, PVt], writes=[ACt[c][tt]])

        AACT = BR4

        def ln(tt):
            p1t, p1a = ps()
            p2t, p2a = ps()
            for c in range(4):
                i = nxt("sq")
                op("act", (lambda i=i, c=c, tt=tt: nc.scalar.activation(SQ[i], ACONV[c][:, tsl(tt)], AF.Copy)),
                   reads=[ACt[c][tt]], writes=[SQt[i]])
                mm(p1t, p1a, ONES, SQ[i], c == 0, c == 3, [SQt[i], Ct])
                i2 = nxt("sq")
                op("act", (lambda i2=i2, c=c, tt=tt: nc.scalar.activation(SQ[i2], ACONV[c][:, tsl(tt)], AF.Square)),
                   reads=[ACt[c][tt]], writes=[SQt[i2]])
                mm(p2t, p2a, ONES, SQ[i2], c == 0, c == 3, [SQt[i2], Ct])
            jm = nxt("rs")
            mean = RSTD[jm]
            op("dve", (lambda mean=mean, p1a=p1a: nc.vector.tensor_single_scalar(mean, p1a, 1.0 / 512, op=ALU.mult)),
               reads=[p1t], writes=[RSTDt[jm]])
            jv = nxt("sg")
            var = SG[jv]
            op("dve", (lambda var=var, mean=mean: nc.vector.tensor_tensor(var, mean, mean, op=ALU.mult)),
               reads=[RSTDt[jm]], writes=[SGt[jv]])
            op("dve", (lambda var=var, p2a=p2a: nc.vector.scalar_tensor_tensor(
                out=var, in0=p2a, scalar=1.0 / 512, in1=var, op0=ALU.mult, op1=ALU.subtract)),
               reads=[p2t, SGt[jv]], writes=[SGt[jv]])
            op("act", (lambda var=var: nc.scalar.activation(var, var, AF.Ln, bias=EPSC, scale=1.0)),
               reads=[SGt[jv], PVt], writes=[SGt[jv]])
            op("act", (lambda var=var: nc.scalar.activation(var, var, AF.Exp, scale=-0.5)),
               reads=[SGt[jv]], writes=[SGt[jv]])
            for c in range(4):
                dst = ACONV[c][:, tsl(tt)]
                op("dve", (lambda dst=dst, mean=mean: nc.vector.tensor_tensor(dst, dst, mean, op=ALU.subtract)),
                   reads=[ACt[c][tt], RSTDt[jm]], writes=[ACt[c][tt]])
                op("dve", (lambda dst=dst, var=var: nc.vector.tensor_tensor(dst, dst, var, op=ALU.mult)),
                   reads=[ACt[c][tt], SGt[jv]], writes=[ACt[c][tt]])
                op("act", (lambda dst=dst, c=c, tt=tt, pb=pb: nc.scalar.activation(
                    AACT[c][:, tsl(tt)], dst, AF.Silu, bias=pcol(pb + 172 + c), scale=pcol(pb + 168 + c))),
                   reads=[ACt[c][tt], PVt], writes=[AAt[c][tt]])

        glu(0)
        glu(1)
        bg_issue(24, upto_layer=l, first_reads=[APt[0][NT]] + flat(HSt))
        conv(0)
        glu(2)
        conv(1)
        glu(3)
        brbase = merged_base(flat(DGt))
        AAt = new_tiles("aact", 4, NT, brbase)
        conv(2, [0])
        conv(3, [0])
        conv(2, [1])
        conv(3, [1])
        ln(0)
        conv(2, [2])
        conv(3, [2])
        ln(1)
        conv(2, [3])
        conv(3, [3])
        ln(2)
        ln(3)
        for c_ in (2, 3):
            _, taps_, si_ = DGc[c_]
            mb_ = merged_base(taps_)
            Wt[si_].w = mb_.w
            Wt[si_].r = {}
        sbase = merged_base(flat(APt))
        MGt = new_tiles("mg", 8, NT, merged_base(flat(ACt)))
        sa, WA = load_kn(w_a_out[l], 0, 1024, k=4)

        def gated_out(gate_c0, branch_fn, first):
            for half in range(2):
                sgt, WG = load_kn(w_in[l], gate_c0 + half * 512, 512)
                for o4 in range(4):
                    oc = half * 4 + o4
                    for tt in range(NT):
                        pbt, pba, scale_col = branch_fn(oc, tt)
                        pgt, pga = ps()
                        for k in range(8):
                            mm(pgt, pga, WG[:, k, o4 * 128:(o4 + 1) * 128], H[k][:, tsl(tt)], k == 0, k == 7,
                               [Wt[sgt], Ht[k][tt]])
                        j = nxt("sg")
                        op("act", (lambda j=j, pga=pga: nc.scalar.activation(SG[j], pga, AF.Sigmoid)),
                           reads=[pgt], writes=[SGt[j]])
                        dst = MG[oc][:, tsl(tt)]
                        if first:
                            op("dve", (lambda j=j, pba=pba, dst=dst: nc.vector.tensor_tensor(dst, pba, SG[j], op=ALU.mult)),
                               reads=[pbt, SGt[j]], writes=[MGt[oc][tt]])
                        else:
                            if scale_col is None:
                                op("dve", (lambda j=j, pba=pba: nc.vector.tensor_tensor(SG[j], pba, SG[j], op=ALU.mult)),
                                   reads=[pbt, SGt[j]], writes=[SGt[j]])
                            else:
                                op("dve", (lambda j=j, pba=pba, sc=scale_col: nc.vector.scalar_tensor_tensor(
                                    out=SG[j], in0=pba, scalar=pcol(sc), in1=SG[j], op0=ALU.mult, op1=ALU.mult)),
                                   reads=[pbt, SGt[j], PVt], writes=[SGt[j]])
                            op("dve", (lambda j=j, dst=dst: nc.vector.tensor_tensor(dst, dst, SG[j], op=ALU.add)),
                               reads=[MGt[oc][tt], SGt[j]], writes=[MGt[oc][tt]])

        def a_branch(oc, tt):
            pt, pa = ps()
            for k in range(4):
                mm(pt, pa, WA[:, k, oc * 128:(oc + 1) * 128], AACT[k][:, tsl(tt)], k == 0, k == 3, [Wt[sa], AAt[k][tt]])
            return pt, pa, None

        gated_out(3072, a_branch, True)
        brbase = merged_base(flat(AAt))

        PPt = new_tiles("pp", 4, NT, brbase)
        PP = BR4
        UPAD = RS[:, 0:16 + T]
        UPt = [Tile("upad%d" % t, sbase) for t in range(NT + 1)]
        LA = RS[:, 2080:2080 + 528]
        LB = RS[:, 2080 + 528:2080 + 1056]
        INVC = RS[:, 3200:3200 + 64]
        RCP = RS[:, 3264:3280]
        LAt = Tile("la", sbase)
        LBt = Tile("lb", sbase)
        IVt = Tile("invc", sbase)
        op("dve", lambda: nc.vector.memset(UPAD[:, 0:16], 0.0), writes=[UPt[0]])
        for jj in range(16):
            op("dve", (lambda jj=jj: nc.vector.memset(RCP[:, jj:jj + 1], 1.0 / (jj + 1))), writes=[IVt])
        for g, w in enumerate((2, 4, 8, 16)):
            op("dve", (lambda g=g, w=w: nc.vector.tensor_single_scalar(INVC[:, g * 16:(g + 1) * 16], RCP, 1.0 / w, op=ALU.max)),
               reads=[IVt], writes=[IVt])
        su, WU = load_kn(w_in[l], 1024, 512)
        for g, w in enumerate((2, 4, 8, 16)):
            nlev = g + 1
            for tt in range(NT):
                pt, pa = ps()
                for k in range(8):
                    mm(pt, pa, WU[:, k, g * 128:(g + 1) * 128], H[k][:, tsl(tt)], k == 0, k == 7, [Wt[su], Ht[k][tt]])
                op("act", (lambda pa=pa, tt=tt: nc.scalar.activation(UPAD[:, 16 + tt * TW:16 + (tt + 1) * TW], pa, AF.Copy)),
                   reads=[pt], writes=[UPt[tt + 1]])
                off = tt * TW
                bufs = [(LA, LAt), (LB, LBt)]
                srcb, srct_ = UPAD[:, off:off + 528], None
                rd = [UPt[tt], UPt[tt + 1]]
                for lev in range(nlev):
                    d = 1 << lev
                    dstb, dstt = bufs[lev % 2]
                    lo = 2 * d
                    op("dve", (lambda dstb=dstb, srcb=srcb, lo=lo, d=d: nc.vector.tensor_tensor(
                        dstb[:, lo:528], srcb[:, lo:528], srcb[:, lo - d:528 - d], op=ALU.add)),
                       reads=rd, writes=[dstt])
                    srcb, rd = dstb, [dstt]
                dst = PP[g][:, tsl(tt)]
                op("dve", (lambda dst=dst, srcb=srcb, w=w, off=off: nc.vector.scalar_tensor_tensor(
                    out=dst, in0=srcb[:, 16:528], scalar=1.0 / w, in1=UPAD[:, off + 16:off + 528],
                    op0=ALU.mult, op1=ALU.subtract)),
                   reads=rd + [UPt[tt + 1]], writes=[PPt[g][tt]])
                if tt == 0:
                    op("dve", (lambda srcb=srcb, g=g: nc.vector.tensor_tensor(
                        srcb[:, 16:32], srcb[:, 16:32], INVC[:, g * 16:(g + 1) * 16], op=ALU.mult)),
                       reads=rd + [IVt], writes=[rd[0]])
                    op("dve", (lambda dst=dst, srcb=srcb: nc.vector.tensor_tensor(
                        dst[:, 0:16], srcb[:, 16:32], UPAD[:, 16:32], op=ALU.subtract)),
                       reads=rd + [UPt[1]], writes=[PPt[g][tt]])
        sbase = merged_base(UPt + [LAt, LBt, IVt])
        sp_i = wslot()
        WP = wview(sp_i, 4, 256)
        wload(sp_i, WP, w_pool[l].rearrange("g p n -> p g n"))

        def b_branch(oc, tt):
            pt, pa = ps()
            g, o2 = oc // 2, oc % 2
            mm(pt, pa, WP[:, g, o2 * 128:(o2 + 1) * 128], PP[g][:, tsl(tt)], True, True, [Wt[sp_i], PPt[g][tt]])
            return pt, pa, pb + 32 + oc

        gated_out(4096, b_branch, False)
        brbase = merged_base(flat(PPt))

        CCt = new_tiles("cc", 4, NT, brbase)
        CC = BR4
        CPAD = RS[:, 0:2 + T]
        CPt = [Tile("cpad%d" % t, sbase) for t in range(NT + 1)]
        CTMP = [RS[:, 2080 + i * 512:2080 + (i + 1) * 512] for i in range(2)]
        CTt = [Tile("ctmp%d" % i, sbase) for i in range(2)]
        op("dve", lambda: nc.vector.memset(CPAD[:, 0:2], 0.0), writes=[CPt[0]])
        sx, WX = load_kn(w_in[l], 1536, 512)
        sc_, WCc = load_kn(w_in[l], 2560, 512)
        sb_, WB = load_kn(w_in[l], 2048, 512)
        for c in range(4):
            for tt in range(NT):
                pxt, pxa = ps()
                for k in range(8):
                    mm(pxt, pxa, WX[:, k, c * 128:(c + 1) * 128], H[k][:, tsl(tt)], k == 0, k == 7, [Wt[sx], Ht[k][tt]])
                pct, pca = ps()
                for k in range(8):
                    mm(pct, pca, WCc[:, k, c * 128:(c + 1) * 128], H[k][:, tsl(tt)], k == 0, k == 7, [Wt[sc_], Ht[k][tt]])
                j = nxt("sg")
                op("act", (lambda j=j, pxa=pxa: nc.scalar.activation(SG[j], pxa, AF.Copy)), reads=[pxt], writes=[SGt[j]])
                op("dve", (lambda j=j, pca=pca, tt=tt: nc.vector.tensor_tensor(
                    CPAD[:, 2 + tt * TW:2 + (tt + 1) * TW], pca, SG[j], op=ALU.mult)),
                   reads=[pct, SGt[j]], writes=[CPt[tt + 1]])
                pbt, pba = ps()
                for k in range(8):
                    mm(pbt, pba, WB[:, k, c * 128:(c + 1) * 128], H[k][:, tsl(tt)], k == 0, k == 7, [Wt[sb_], Ht[k][tt]])
                ci = (c * NT + tt) % 2
                wc = pb + 176 + c * 3
                rd = [CPt[tt], CPt[tt + 1], PVt]
                op("dve", (lambda ci=ci, tt=tt, wc=wc: nc.vector.tensor_single_scalar(
                    CTMP[ci], CPAD[:, tt * TW:tt * TW + TW], pcol(wc), op=ALU.mult)),
                   reads=rd, writes=[CTt[ci]])
                for k in (1, 2):
                    op("dve", (lambda ci=ci, tt=tt, wc=wc, k=k: nc.vector.scalar_tensor_tensor(
                        out=CTMP[ci], in0=CPAD[:, tt * TW + k:tt * TW + k + TW], scalar=pcol(wc + k), in1=CTMP[ci],
                        op0=ALU.mult, op1=ALU.add)),
                       reads=rd + [CTt[ci]], writes=[CTt[ci]])
                op("dve", (lambda ci=ci, c=c, tt=tt, pba=pba: nc.vector.tensor_tensor(
                    CC[c][:, tsl(tt)], pba, CTMP[ci], op=ALU.mult)),
                   reads=[pbt, CTt[ci]], writes=[CCt[c][tt]])
        sbase = merged_base(CPt + CTt)
        sco, WCO = load_kn(w_c_out[l], 0, 1024, k=4)

        def c_branch(oc, tt):
            pt, pa = ps()
            for k in range(4):
                mm(pt, pa, WCO[:, k, oc * 128:(oc + 1) * 128], CC[k][:, tsl(tt)], k == 0, k == 3, [Wt[sco], CCt[k][tt]])
            return pt, pa, None

        gated_out(5120, c_branch, False)
        brbase = merged_base(flat(CCt))
        hbase = merged_base(flat(Ht))

        def proj_add(wap, SRC, SRCt, gcol, base_fn, after_loads=None, Wh=None):
            if Wh is None:
                Wh = [load_kn(wap, half * 512, 512) for half in range(2)]
            if after_loads is not None:
                after_loads()
            Hn = [[None] * NT for _ in range(8)]
            for tt in range(NT):
                for oc in range(8):
                    s_, Wv_ = Wh[oc // 4]
                    o4 = oc % 4
                    pt, pa = ps()
                    for k in range(8):
                        mm(pt, pa, Wv_[:, k, o4 * 128:(o4 + 1) * 128], SRC[k][:, tsl(tt)], k == 0, k == 7,
                           [Wt[s_], SRCt[k][tt]])
                    dst = X[oc][:, tsl(tt)]
                    op("dve", (lambda dst=dst, pa=pa: nc.vector.tensor_tensor(dst, dst, pa, op=ALU.add)),
                       reads=[pt, Xt[oc][tt]], writes=[Xt[oc][tt]])
                for c in range(8):
                    Hn[c][tt] = Tile("h%d_%d" % (c, tt), base_fn(c, tt))
                norm_tile(tt, gcol, Hn)
            return Hn

        MEMT = RS[:, 0:2048].rearrange("p (k n) -> p k n", k=8)
        MEMTt = Tile("memt", sbase)
        op("sp", lambda: nc.sync.dma_start(out=MEMT, in_=memT.rearrange("(k p) n -> p k n", p=128)),
           writes=[MEMTt], dma="mem")
        MEMN = BRB[:, 4096:6144].rearrange("p (k n) -> p k n", k=8)
        MEMNt = Tile("memn", brbase)
        KT = BRB[:, 0:2048].rearrange("p (k n) -> p k n", k=8)
        KTt = Tile("kt", brbase)
        VV = BRB[:, 2048:4096].rearrange("p (k n) -> p k n", k=2)
        VVt = Tile("vv", brbase)
        memsrc = [MEMT[:, c, :] for c in range(8)]
        (rt, rs), = rms_stats(memsrc, [[MEMTt]] * 8, NMEM, [(slice(0, NMEM), 0)])
        for c in range(8):
            op("dve", (lambda c=c, rs=rs, pb=pb: nc.vector.scalar_tensor_tensor(
                out=MEMN[:, c, :], in0=MEMT[:, c, :], scalar=pcol(pb + 24 + c), in1=rs, op0=ALU.mult, op1=ALU.mult)),
               reads=[MEMTt, rt, PVt], writes=[MEMNt])
        for half in range(2):
            s_, Wk = load_kn(w_xkv[l], half * 512, 512)
            for o4 in range(4):
                dc = half * 4 + o4
                pt, pa = ps()
                pa = pa[:, 0:NMEM]
                for k in range(8):
                    mm(pt, pa, Wk[:, k, o4 * 128:(o4 + 1) * 128], MEMN[:, k, :], k == 0, k == 7, [Wt[s_], MEMNt])
                op("act", (lambda dc=dc, pa=pa: nc.scalar.activation(KT[:, dc, :], pa, AF.Copy)), reads=[pt], writes=[KTt])
        for half in range(2):
            s_, Wvv = load_kn(w_xkv[l], 1024 + half * 512, 512)
            for mc in range(2):
                pt, pa = ps()
                for k in range(8):
                    mm(pt, pa, MEMN[:, k, mc * 128:(mc + 1) * 128], Wvv[:, k, :], k == 0, k == 7, [Wt[s_], MEMNt])
                op("act", (lambda mc=mc, half=half, pa=pa: nc.scalar.activation(
                    VV[:, mc, half * 512:(half + 1) * 512], pa, AF.Copy)), reads=[pt], writes=[VVt])
        sbase = merged_base([MEMTt])
        hb_ = hbase
        Ht = proj_add(w_o[l], MG, MGt, pb + 8, lambda c, tt: hb_)
        mgbase = merged_base(flat(MGt))
        if stage == ("mix", l):
            break
        if ("postmix", l) in skip:
            continue

        Qt = new_tiles("q", 8, NT, mgbase)
        Q = MG
        for half in range(2):
            s_, Wq = load_kn(w_xq[l], half * 512, 512)
            for o4 in range(4):
                dc = half * 4 + o4
                for tt in range(NT):
                    pt, pa = ps()
                    for k in range(8):
                        mm(pt, pa, Wq[:, k, o4 * 128:(o4 + 1) * 128], H[k][:, tsl(tt)], k == 0, k == 7, [Wt[s_], Ht[k][tt]])
                    op("act", (lambda dc=dc, tt=tt, pa=pa: nc.scalar.activation(Q[dc][:, tsl(tt)], pa, AF.Copy)),
                       reads=[pt], writes=[Qt[dc][tt]])
        Wh_xo = [load_kn(w_xo[l], half * 512, 512) for half in range(2)]
        bg_issue(1000, upto_layer=l)
        hbase = merged_base(flat(Ht))
        Ot = new_tiles("o", 8, NT, hbase)
        O = H
        EE = [bfv(RS[:, i * 512:(i + 1) * 512]).rearrange("p (k n) -> p k n", k=2) for i in range(3)]
        EEt = [[Tile("ee%d_%d" % (i, m), sbase) for m in range(2)] for i in range(3)]
        RD = [RS[:, 1536 + i * 512:1536 + (i + 1) * 512] for i in range(2)]
        RDt = [Tile("rd%d" % i, sbase) for i in range(2)]

        def att_S(u, tt, h):
            ei = u % 3
            for mc in range(2):
                pt, pa = ps()
                for j in range(2):
                    mm(pt, pa, KT[:, 2 * h + j, mc * 128:(mc + 1) * 128], Q[2 * h + j][:, tsl(tt)], j == 0, j == 1,
                       [KTt, Qt[2 * h + j][tt]])
                op("act", (lambda ei=ei, mc=mc, pa=pa: nc.scalar.activation(EE[ei][:, mc, :], pa, AF.Exp, scale=1.0 / 16)),
                   reads=[pt], writes=[EEt[ei][mc]])

        def att_D(u, tt, h):
            ei = u % 3
            ri = u % 2
            pdt, pda = ps()
            for mc in range(2):
                mm(pdt, pda, ONES, EE[ei][:, mc, :], mc == 0, mc == 1, [EEt[ei][mc], Ct])
            op("act", (lambda ri=ri, pda=pda: nc.scalar.activation(RD[ri], pda, AF.Ln)), reads=[pdt], writes=[RDt[ri]])
            op("act", (lambda ri=ri: nc.scalar.activation(RD[ri], RD[ri], AF.Exp, scale=-1.0)), reads=[RDt[ri]], writes=[RDt[ri]])
            for j in range(2):
                dc = 2 * h + j
                pt, pa = ps()
                for mc in range(2):
                    mm(pt, pa, VV[:, mc, dc * 128:(dc + 1) * 128], EE[ei][:, mc, :], mc == 0, mc == 1, [VVt, EEt[ei][mc]])
                op("dve", (lambda dc=dc, tt=tt, pa=pa, ri=ri: nc.vector.tensor_tensor(
                    O[dc][:, tsl(tt)], pa, RD[ri], op=ALU.mult)),
                   reads=[pt, RDt[ri]], writes=[Ot[dc][tt]])

        aunits = [(tt, h) for tt in range(NT) for h in range(4)]
        for u, (tt, h) in enumerate(aunits):
            att_S(u, tt, h)
            if u > 0:
                att_D(u - 1, *aunits[u - 1])
        att_D(len(aunits) - 1, *aunits[-1])
        sbase = merged_base(flat(EEt) + RDt)
        mgbase = merged_base(flat(Qt))
        brbase = merged_base([MEMNt, KTt, VVt])
        WGUb = [bfv(RMG[:, i * 2048:(i + 1) * 2048]) for i in range(4)]
        WGt = [Tile("wg%d" % i, mgbase) for i in range(4)]
        WUt = [Tile("wu%d" % i, mgbase) for i in range(4)]
        def prefetch_unit0(l=l):
            if not (stage is None or stage[0] not in ("mix", "xattn")):
                return
            bg_issue(1000, upto_layer=l)
            for j in range(4):
                r0 = (l * NE + j) * 128
                op("pool", (lambda j=j, r0=r0: nc.gpsimd.dma_start(out=WGUb[j][:, 0:2048], in_=wbf_g[r0:r0 + 128, :])),
                   reads=PCt[l], writes=[WGt[j]], dma="wg%d" % j)
                op("pool", (lambda j=j, r0=r0: nc.gpsimd.dma_start(out=WGUb[j][:, 2048:4096], in_=wbf_u[r0:r0 + 128, :])),
                   reads=PCt[l], writes=[WUt[j]], dma="wu%d" % j)
        Ht = proj_add(w_xo[l], O, Ot, pb + 16, lambda c, tt: Ot[c][tt], after_loads=prefetch_unit0, Wh=Wh_xo)
        if stage == ("xattn", l):
            break
        if ("moe", l) in skip:
            continue

        sr = wslot()
        WR = wview(sr, 8, 20)
        wload(sr, WR, w_r[l].rearrange("(k p) n -> p k n", p=128))
        AX = mybir.AxisListType.X
        rt_ = Tile("router", sbase)

        def rs_(a, n):
            return RS[:, a:a + n]
        LG = rs_(0, 320).rearrange("p (t j) -> p t j", j=20)
        Dm_f, ED_f, GS_f, LES_f, LES2_f, M1_f, M2_f, TM_f, GL_f = [rs_(320 + 64 * i, 64) for i in range(9)]
        v3 = lambda ap: ap.rearrange("p (t j) -> p t j", j=4)
        Dm, ED, GSEL, LES, LES2, M1, M2, TM, GL = [v3(x_) for x_ in (Dm_f, ED_f, GS_f, LES_f, LES2_f, M1_f, M2_f, TM_f, GL_f)]
        sm = [rs_(896 + 16 * i, 16) for i in range(12)]
        GG = rs_(1088, 256)
        GG4 = GG.rearrange("p (t j i) -> p t j i", j=4, i=4)
        GD = rs_(1344, 256)
        GHI = bfv(rs_(1600, 128))
        GLO = bfv(rs_(1728, 128))
        LP = rs_(1344, 256).rearrange("p (t j i) -> p t j i", j=4, i=4)

        def dv(fn, rd=()):
            op("dve", fn, reads=[rt_] + list(rd), writes=[rt_])

        def bc3(ap):
            return ap.unsqueeze(2).to_broadcast([128, 16, 4])
        TT_ = nc.vector.tensor_tensor
        RED = nc.vector.tensor_reduce
        plt, pla = ps()
        for tc in range(16):
            for k in range(8):
                mm(plt, pla[:, tc * 20:(tc + 1) * 20], H[k][:, tc * 128:(tc + 1) * 128], WR[:, k, :], k == 0, k == 7,
                   [Wt[sr], Ht[k][tc // 4]])
        ROW = ([bfv(RBR[:, 2048 + i * 520:2048 + (i + 1) * 520]) for i in range(2)]
               + [bfv(RBR[:, i * 520:(i + 1) * 520]) for i in range(3)]
               + [bfv(RW[3][:, i * 520:(i + 1) * 520]) for i in range(3)])
        ROWK = [r_[:, 0:1024].rearrange("p (j k) -> p k j", k=8) for r_ in ROW]
        ROWt = ([Tile("row%d" % i, brbase) for i in range(5)] + [Tile("row%d" % (5 + i), Wt[3]) for i in range(3)])

        def stage_rows(tc):
            i = tc % 8
            pt, pa = ps()
            pbf = pa.bitcast(BF16)
            for k in range(8):
                op("pe", (lambda pbf=pbf, k=k, tc=tc: nc.tensor.transpose(
                    pbf[:, k * 128:(k + 1) * 128], H[k][:, tc * 128:(tc + 1) * 128], IDENT)),
                   reads=[Ht[k][tc // 4], Ct], writes=[pt])
            op("act", (lambda i=i, pbf=pbf: nc.scalar.activation(ROW[i][:, 0:1024], pbf, AF.Copy)), reads=[pt], writes=[ROWt[i]])

        dv((lambda pla=pla, pb=pb: TT_(LG, pla[:, 0:320].rearrange("p (t j) -> p t j", j=20),
                                        PV[:, pb + 188:pb + 208].unsqueeze(1).to_broadcast([128, 16, 20]), op=ALU.add)), [plt, PVt])
        for tc in range(8):
            stage_rows(tc)
        gmax, gden, gval, l1, l2, dd, ed, w1, w2, tmp = sm[:10]
        LE = LG[:, :, 4:20].rearrange("p t (j i) -> p t j i", i=4)
        dv(lambda: RED(out=gmax, in_=LG[:, :, 0:4], axis=AX, op=ALU.max))
        dv(lambda: TT_(Dm, LG[:, :, 0:4], bc3(gmax), op=ALU.subtract))
        op("act", lambda: nc.scalar.activation(ED_f, Dm_f, AF.Exp), reads=[rt_], writes=[rt_])
        dv(lambda: RED(out=gden, in_=ED, axis=AX, op=ALU.add))
        dv(lambda: nc.vector.reciprocal(gval, gden))
        dv(lambda: nc.vector.tensor_single_scalar(GS_f, Dm_f, 0.0, op=ALU.is_ge))
        dv(lambda: TT_(LP, LE, GSEL.unsqueeze(3).to_broadcast([128, 16, 4, 4]), op=ALU.mult))
        dv(lambda: RED(out=LES, in_=LP.rearrange("p t j i -> p t i j"), axis=AX, op=ALU.add))
        dv(lambda: RED(out=l1, in_=LES, axis=AX, op=ALU.max))
        dv(lambda: TT_(TM, LES, bc3(l1), op=ALU.subtract))
        dv(lambda: nc.vector.tensor_single_scalar(M1_f, TM_f, 0.0, op=ALU.is_ge))
        dv(lambda: nc.vector.scalar_tensor_tensor(out=LES2_f, in0=M1_f, scalar=-1e30, in1=LES_f, op0=ALU.mult, op1=ALU.add))
        dv(lambda: RED(out=l2, in_=LES2, axis=AX, op=ALU.max))
        dv(lambda: TT_(TM, LES2, bc3(l2), op=ALU.subtract))
        dv(lambda: nc.vector.tensor_single_scalar(M2_f, TM_f, 0.0, op=ALU.is_ge))
        dv(lambda: TT_(dd, l2, l1, op=ALU.subtract))
        op("act", lambda: nc.scalar.activation(ed, dd, AF.Exp), reads=[rt_], writes=[rt_])
        dv(lambda: nc.vector.tensor_single_scalar(tmp, ed, 1.0, op=ALU.add))
        dv(lambda: nc.vector.reciprocal(w1, tmp))
        dv(lambda: TT_(w2, ed, w1, op=ALU.mult))
        dv(lambda: TT_(w1, w1, gval, op=ALU.mult))
        dv(lambda: TT_(w2, w2, gval, op=ALU.mult))
        dv(lambda: TT_(GL, M1, bc3(w1), op=ALU.mult))
        dv(lambda: TT_(TM, M2, bc3(w2), op=ALU.mult))
        dv(lambda: TT_(GL_f, GL_f, TM_f, op=ALU.add))
        if stage == ("b0r", l):
            break
        c0 = 2 * NPL + 9
        I32v = lambda a_, n_: RS[:, a_:a_ + n_].bitcast(I32)
        POSI = I32v(3584, 16)
        POSF = rs_(3600, 16)
        NGc, NTc, BASE = rs_(3616, 4), rs_(3620, 4), rs_(3624, 4)
        GUF = rs_(3632, 7)
        CMP = rs_(3640, 16).rearrange("p (g j) -> p g j", j=4)
        CMP2 = rs_(3656, 21).rearrange("p (u g) -> p u g", g=3)
        T28 = rs_(3680, 28).rearrange("p (u j) -> p u j", j=4)
        IDX1 = I32v(3708, 28)
        IDX2 = I32v(3736, 28)
        EXC = rs_(3764, 64).rearrange("p (t g) -> p t g", g=4)
        PGF_f = rs_(3828, 64)
        PGF = PGF_f.rearrange("p (t g) -> p t g", g=4)
        GSB = bfv(rs_(3892, 32))
        GHL = bfv(rs_(3924, 128)).rearrange("p (t c) -> p t c", c=16)
        GSB3 = GSB.rearrange("p (t g) -> p t g", g=4)
        dv(lambda: nc.vector.tensor_copy(GSB, GS_f))
        ppt, ppa = PSt[2], PSB[2]
        mm(ppt, ppa[:, 0:64], UT, GSB, True, True, [rt_, UTt])
        mm(ppt, ppa[:, 64:128], ONES, GSB, True, True, [rt_, Ct])
        CNTp = ppa[:, 64:128].rearrange("p (t g) -> p t g", g=4)
        dv(lambda: nc.vector.memset(EXC[:, 0, :], 0.0))
        for tc in range(15):
            dv((lambda tc=tc: TT_(EXC[:, tc + 1, :], EXC[:, tc, :], CNTp[:, tc, :], op=ALU.add)), [ppt])
        if stage == ("s0", l):
            break
        dv((lambda: TT_(NGc, EXC[:, 15, :], CNTp[:, 15, :], op=ALU.add)), [ppt])
        if stage == ("s1", l):
            break
        dv((lambda: TT_(CMP, NGc.unsqueeze(2).to_broadcast([128, 4, 4]),
                        PV[:, c0 + 1:c0 + 5].unsqueeze(1).to_broadcast([128, 4, 4]), op=ALU.is_gt)), [PVt])
        dv(lambda: RED(out=NTc, in_=CMP, axis=AX, op=ALU.add))
        dv(lambda: nc.vector.tensor_single_scalar(NTc[:, 0:1], NTc[:, 0:1], 1.0, op=ALU.max))
        dv(lambda: nc.vector.memset(BASE[:, 0:1], 0.0))
        dv(lambda: nc.vector.tensor_copy(BASE[:, 1:2], NTc[:, 0:1]))
        dv(lambda: TT_(BASE[:, 2:3], BASE[:, 1:2], NTc[:, 1:2], op=ALU.add))
        dv(lambda: TT_(BASE[:, 3:4], BASE[:, 2:3], NTc[:, 2:3], op=ALU.add))
        if stage == ("s2", l):
            break
        dv((lambda: TT_(PGF, ppa[:, 0:64].rearrange("p (t g) -> p t g", g=4), EXC, op=ALU.add)), [ppt])
        dv(lambda: nc.vector.scalar_tensor_tensor(out=PGF, in0=BASE.unsqueeze(1).to_broadcast([128, 16, 4]), scalar=512.0,
                                                  in1=PGF, op0=ALU.mult, op1=ALU.add))
        dv(lambda: TT_(PGF_f, PGF_f, GS_f, op=ALU.mult))
        dv(lambda: RED(out=POSF, in_=PGF, axis=AX, op=ALU.add))
        if stage == ("s3", l):
            break
        dv(lambda: nc.vector.tensor_copy(POSI, POSF))
        POSH = I32v(3736, 16)
        if l > 0:
            dv(lambda l=l: nc.vector.tensor_single_scalar(POSF, POSF, float(l * NSLOT), op=ALU.add))
        dv(lambda: nc.vector.tensor_copy(POSH, POSF))
        if stage == ("b0p", l):
            break
        dv((lambda: TT_(CMP2, PV[:, c0 + 5:c0 + 12].unsqueeze(2).to_broadcast([128, 7, 3]),
                        BASE[:, 1:4].unsqueeze(1).to_broadcast([128, 7, 3]), op=ALU.is_ge)), [PVt])
        dv(lambda: RED(out=GUF, in_=CMP2, axis=AX, op=ALU.add))
        dv((lambda: nc.vector.scalar_tensor_tensor(out=T28, in0=GUF.unsqueeze(2).to_broadcast([128, 7, 4]), scalar=512.0,
                                                   in1=PV[:, c0 + 12:c0 + 16].unsqueeze(1).to_broadcast([128, 7, 4]),
                                                   op0=ALU.mult, op1=ALU.add)), [PVt])
        if l > 0:
            dv(lambda l=l: nc.vector.tensor_single_scalar(rs_(3680, 28), rs_(3680, 28), l * 2048.0, op=ALU.add))
        dv(lambda: nc.vector.tensor_copy(IDX1, T28.rearrange("p u j -> p (u j)")))
        if stage == ("b0i", l):
            break
        dv(lambda: nc.vector.memset(GHL, 0.0))
        dv(lambda: nc.vector.tensor_copy(GHL[:, :, 0:4], GL))
        dv(lambda: TT_(TM, GL, GHL[:, :, 0:4], op=ALU.subtract))
        dv(lambda: nc.vector.tensor_copy(GHL[:, :, 4:8], TM))

        HG = [bfv(RW[i]).rearrange("p (k n) -> p k n", k=8) for i in range(2)]
        HGt = [Tile("hg%d" % i, Wt[i]) for i in range(2)]
        ACTB = [bfv(RW[2 + i]).rearrange("p (k n) -> p k n", k=8) for i in range(2)]
        ACTBt = [[Tile("actb%d_%d" % (0, q), Wt[2]) for q in range(8)], None]
        YROW = [RBR[:, i * 1024:(i + 1) * 1024] for i in range(2)]
        SEL8 = bfv(RBR[0:16, 3088:3600])
        SEL8t = Tile("sel8", brbase)
        op("pool", lambda: nc.gpsimd.dma_start(out=SEL8, in_=cst[0:16, 128:128 + 1024]), writes=[SEL8t], dma="sel")
        GB = [RS[:, 2048 + i * 512:2048 + (i + 1) * 512] for i in range(2)]
        GBt = [Tile("gb%d" % i, sbase) for i in range(2)]
        GT = [bfv(RS[0:16, 3072 + i * 256:3072 + (i + 1) * 256]) for i in range(2)]
        GTt = [Tile("gt%d" % i, sbase) for i in range(2)]
        nrw = (l + 1) * NE * 128
        weg2, weu2, wed2 = wbf_g[0:nrw, :], wbf_u[0:nrw, :], wbf_d[0:nrw, :]
        IOA = bass.IndirectOffsetOnAxis
        wgu_of = {}

        def load_wgu(u, j):
            si = j
            c = u * 4 + j
            op("pool", (lambda si=si, c=c: nc.gpsimd.indirect_dma_start(
                out=WGUb[si][:, 0:2048], out_offset=None, in_=weg2, in_offset=IOA(ap=IDX1[:, c:c + 1], axis=0))),
               reads=[rt_] + PCt[l], after=[WZt], writes=[WGt[si]], dma="wg%d" % si)
            op("pool", (lambda si=si, c=c: nc.gpsimd.indirect_dma_start(
                out=WGUb[si][:, 2048:4096], out_offset=None, in_=weu2, in_offset=IOA(ap=IDX1[:, c:c + 1], axis=0))),
               reads=[rt_] + PCt[l], after=[WZt], writes=[WUt[si]], dma="wu%d" % si)
            wgu_of[(u, j)] = si

        if stage == ("b1", l):
            break
        for j in range(4):
            wgu_of[(0, j)] = j
        if stage == ("b1w", l):
            break

        rowc = [0]

        def send_rows(tc):
            i = tc % 8
            op("dve", (lambda i=i, tc=tc: nc.vector.tensor_copy(ROW[i][:, 1024:1040], GHL[:, tc, :])), reads=[rt_], writes=[ROWt[i]])
            op("pool", (lambda i=i, tc=tc, l=l: nc.gpsimd.indirect_dma_start(
                out=hs_all, out_offset=IOA(ap=POSH[:, tc:tc + 1], axis=0), in_=ROW[i], in_offset=None)),
               reads=[ROWt[i], rt_], writes=[HSt[l][i]], dma="hs%d" % i)

        for tc in range(8):
            send_rows(tc)
        for tc in range(8, 16):
            stage_rows(tc)
            send_rows(tc)
        WDUf = [bfv(RH[:, i * 4096:(i + 1) * 4096]) for i in range(2)]
        WDU = [w_.rearrange("p (q n) -> p q n", q=8) for w_ in WDUf]
        hdead = merged_base(flat(Ht))
        WDt = [[Tile("wd%d_%d" % (i, j), hdead) for j in range(4)] for i in range(2)]

        def load_wd(u, j):
            ub = u % 2
            c = u * 4 + j
            op("pool", (lambda ub=ub, j=j, c=c: nc.gpsimd.indirect_dma_start(
                out=WDUf[ub][:, 2 * j * 1024:(2 * j + 2) * 1024], out_offset=None, in_=wed2,
                in_offset=IOA(ap=IDX1[:, c:c + 1], axis=0))),
               reads=[rt_] + PCt[l], after=[WZt], writes=[WDt[ub][j]], dma="wd%d_%d" % (ub, j))

        if stage == ("b2s", l):
            break
        for j in range(4):
            load_wd(0, j)
        if stage == ("b2", l):
            break

        def CU(u, part=None):
            ub = u % 2
            pgt, pga = ps()
            pgb = pga.bitcast(BF16)
            s4s = list(range(4) if part is None else range(2 * part, 2 * part + 2))
            for s4 in s4s:
                i = (rowc[0] % 2) if u > 0 else (2 + s4)
                rowc[0] += 1
                r0 = u * 512 + s4 * 128
                op("sp", (lambda i=i, r0=r0, l=l: nc.sync.dma_start(out=ROW[i], in_=hs_all[l * NSLOT + r0:l * NSLOT + r0 + 128, :])),
                   reads=HSt[l], writes=[ROWt[i]], dma="hl%d" % i)
                pt, pa = ps()
                pbf = pa.bitcast(BF16)
                for k in range(8):
                    op("pe", (lambda pbf=pbf, k=k, i=i: nc.tensor.transpose(pbf[:, k * 128:(k + 1) * 128], ROWK[i][:, k, :], IDENT)),
                       reads=[ROWt[i], Ct], writes=[pt])
                op("pe", (lambda pgb=pgb, s4=s4, i=i: nc.tensor.transpose(pgb[0:16, s4 * 128:(s4 + 1) * 128], ROW[i][:, 1024:1040], IDENT)),
                   reads=[ROWt[i], Ct], writes=[pgt])
                op("act", (lambda ub=ub, s4=s4, pbf=pbf: nc.scalar.activation(
                    HG[ub][:, :, s4 * 128:(s4 + 1) * 128], pbf.rearrange("p (k n) -> p k n", k=8), AF.Copy)),
                   reads=[pt], writes=[HGt[ub]])
            c_lo, c_hi = s4s[0] * 128, (s4s[-1] + 1) * 128
            op("act", (lambda ub=ub, pgb=pgb, c_lo=c_lo, c_hi=c_hi: nc.scalar.activation(
                GT[ub][:, c_lo:c_hi], pgb[0:16, c_lo:c_hi], AF.Copy)), reads=[pgt], writes=[GTt[ub]])

        gbc = [0]

        def GUS(u, j):
            ub = u % 2
            si = wgu_of[(u, j)]
            WG = WGUb[si][:, 0:2048].rearrange("p (k n) -> p k n", k=8)
            WU = WGUb[si][:, 2048:4096].rearrange("p (k n) -> p k n", k=8)
            pGt, pGa = ps()
            mm(pGt, pGa, SEL8[:, j * 128:(j + 1) * 128], GT[ub], True, False, [SEL8t, GTt[ub]])
            mm(pGt, pGa, SEL8[:, (4 + j) * 128:(5 + j) * 128], GT[ub], False, True, [SEL8t, GTt[ub]])
            gi = gbc[0] % 2
            gbc[0] += 1
            op("act", (lambda gi=gi, pGa=pGa: nc.scalar.activation(GB[gi], pGa, AF.Copy)), reads=[pGt], writes=[GBt[gi]])
            for f in range(2):
                pgt, pga = ps()
                for k in range(8):
                    mm(pgt, pga, WG[:, k, f * 128:(f + 1) * 128], HG[ub][:, k, :], k == 0, k == 7, [WGt[si], HGt[ub]])
                put, pua = ps()
                for k in range(8):
                    mm(put, pua, WU[:, k, f * 128:(f + 1) * 128], HG[ub][:, k, :], k == 0, k == 7, [WUt[si], HGt[ub]])
                jj = nxt("sg")
                op("act", (lambda jj=jj, pga=pga: nc.scalar.activation(SG[jj], pga, AF.Silu)), reads=[pgt], writes=[SGt[jj]])
                op("dve", (lambda jj=jj, pua=pua: nc.vector.tensor_tensor(SG[jj], SG[jj], pua, op=ALU.mult)),
                   reads=[put, SGt[jj]], writes=[SGt[jj]])
                q = 2 * j + f
                op("dve", (lambda jj=jj, gi=gi, ub=ub, q=q: nc.vector.tensor_tensor(ACTB[ub][:, q, :], SG[jj], GB[gi], op=ALU.mult)),
                   reads=[GBt[gi], SGt[jj]], writes=[ACTBt[ub][q]])

        yc = [0]

        def DOWNS(u):
            ub = u % 2
            for s4 in range(4):
                yi = yc[0] % 2
                yc[0] += 1
                for half in range(2):
                    pt, pa = ps()
                    for q in range(8):
                        mm(pt, pa, ACTB[ub][:, q, s4 * 128:(s4 + 1) * 128], WDU[ub][:, q, half * 512:(half + 1) * 512],
                           q == 0, q == 7, [ACTBt[ub][q], WDt[ub][q // 2]])
                    dst = YROW[yi][:, half * 512:(half + 1) * 512]
                    if half == 0:
                        op("act", (lambda dst=dst, pa=pa: nc.scalar.activation(dst, pa, AF.Copy)), reads=[pt], writes=[YROWt[yi]])
                    else:
                        op("dve", (lambda dst=dst, pa=pa: nc.vector.tensor_copy(dst, pa)), reads=[pt], writes=[YROWt[yi]])
                r0 = u * 512 + s4 * 128
                op("sp", (lambda yi=yi, r0=r0: nc.sync.dma_start(out=ys_d[r0:r0 + 128, :], in_=YROW[yi])),
                   reads=[YROWt[yi]], writes=[YSt[yi]], dma="ys%d" % yi)

        last_layer = (l == (layers[-1] if layers is not None else n_layers - 1))
        fuse_next = (stage is None)
        Hnx = [[None] * NT for _ in range(8)]
        CU(0)
        YROWt = [Tile("yrow%d" % i, merged_base(ROWt[2:5])) for i in range(2)]
        ACTBt[1] = [Tile("actb%d_%d" % (1, q), merged_base(ROWt[5:8])) for q in range(8)]
        for u in range(NU):
            for j in range(4):
                if j == 3 and u + 1 < NU:
                    CU(u + 1, 0)
                GUS(u, j)
                if u + 1 < NU:
                    load_wgu(u + 1, j)
            if u + 1 < NU:
                CU(u + 1, 1)
            if u >= 1:
                DOWNS(u - 1)
            if u + 1 < NU:
                for j in range(4):
                    load_wd(u + 1, j)
        DOWNS(NU - 1)
        if stage == ("b3", l):
            break
        preloaded = False
        if fuse_next and not last_layer:
            for i_, tl_ in ((3, ACTBt[1]), (2, ACTBt[0])):
                mb_ = merged_base(tl_)
                Wt[i_].w = mb_.w
                Wt[i_].r = {}
            pre_w[0] = (load_kn(w_in[l + 1], 0, 512, slot=3), load_kn(w_in[l + 1], 512, 512, slot=2))
            wctr[0] = 0
            preloaded = True

        IDF = RS[:, 3072:3200]
        IDFt = Tile("idf", merged_base(GTt))
        op("sp", lambda: nc.sync.dma_start(out=IDF, in_=cst[:, 0:128]), writes=[IDFt], dma="idf")
        YT = ([RW[1][:, i * 1024:(i + 1) * 1024] for i in range(2)] + [RW[0][:, i * 1024:(i + 1) * 1024] for i in range(2)]
              + [RS[:, 2048:3072]])
        YTt = ([Tile("yt%d" % i, HGt[1]) for i in range(2)] + [Tile("yt%d" % (2 + i), HGt[0]) for i in range(2)]
               + [Tile("yt4", merged_base(GBt))])
        obbase[0] = merged_base([rt_])
        hb2 = merged_base(flat(WDt))
        ygc = [0]
        for tt in range(NT):
            banks = [ps() for _ in range(8)]
            for tci in range(4):
                tc = tt * 4 + tci
                b_ = ygc[0] % 5
                ygc[0] += 1
                op("pool", (lambda b_=b_, tc=tc: nc.gpsimd.indirect_dma_start(
                    out=YT[b_], out_offset=None, in_=ys_d, in_offset=IOA(ap=POSI[:, tc:tc + 1], axis=0))),
                   reads=YSt + [rt_], writes=[YTt[b_]], dma="yg%d" % b_)
                for oc in range(8):
                    pt, pa = banks[oc]
                    op("pe", (lambda pa=pa, tci=tci, b_=b_, oc=oc: nc.tensor.transpose(
                        pa[:, tci * 128:(tci + 1) * 128], YT[b_][:, oc * 128:(oc + 1) * 128], IDF)),
                       reads=[YTt[b_], IDFt], writes=[pt])
            for oc in range(8):
                pt, pa = banks[oc]
                dst = X[oc][:, tsl(tt)]
                op("dve", (lambda dst=dst, pa=pa: nc.vector.tensor_tensor(dst, dst, pa, op=ALU.add)),
                   reads=[pt, Xt[oc][tt]], writes=[Xt[oc][tt]])
            if fuse_next:
                if not last_layer:
                    for c in range(8):
                        Hnx[c][tt] = Tile("h%d_%d" % (c, tt), hb2)
                    norm_tile(tt, (l + 1) * NPL, Hnx)
                else:
                    final_tile(tt)
        mgbase = merged_base(WGt + WUt)
        sbase = merged_base([rt_] + GBt + GTt + [IDFt, YTt[4]])
        brbase = merged_base(YROWt + ROWt[0:5] + [SEL8t])
        hbase = hb2
        for i_, tl_ in ((0, [HGt[0]] + YTt[2:4]), (1, [HGt[1]] + YTt[0:2]), (2, ACTBt[0]), (3, ACTBt[1])):
            if preloaded and i_ >= 2:
                continue
            mb_ = merged_base(tl_)
            Wt[i_].w = mb_.w
            Wt[i_].r = {}
        Ht_pre = Hnx if (fuse_next and not last_layer) else None
        if stage == ("moe", l):
            break

    if stage is not None:
        for c in range(8):
            ev = op("sp", (lambda c=c: nc.sync.dma_start(out=outT[c * 128:(c + 1) * 128, :], in_=X[c])),
                    reads=Xt[c], dma="o%d" % (c % 4))
            final.append(ev)
    last = {}
    for k, v in final:
        last[k] = max(last.get(k, 0), v)
    P.emit(list(last.items()))
    return nc, P


def _cols(v):
    return np.ascontiguousarray(np.asarray(v, np.float32).reshape(-1, 128).T)


def make_shared(inp):
    g = lambda k: np.asarray(inp[k], np.float32)
    pvn = np.zeros((128, NPV), np.float32)
    for l in range(L):
        b = l * NPL
        pvn[:, b + 0:b + 8] = _cols(g("norm_mix_g")[l])
        pvn[:, b + 8:b + 16] = _cols(g("norm_x_g")[l])
        pvn[:, b + 16:b + 24] = _cols(g("norm_ffn_g")[l])
        pvn[:, b + 24:b + 32] = _cols(g("norm_mem_g")[l])
        pvn[:, b + 32:b + 40] = _cols(g("pool_scale")[l])
        caw = g("conv_a_w")[l]
        for c in range(4):
            pvn[:, b + 40 + c * 31:b + 40 + (c + 1) * 31] = caw[:, c * 128:(c + 1) * 128].T
        pvn[:, b + 164:b + 168] = _cols(g("conv_a_b")[l])
        pvn[:, b + 168:b + 172] = _cols(g("ln_a_g")[l])
        pvn[:, b + 172:b + 176] = _cols(g("ln_a_b")[l])
        ccw = g("conv_c_w")[l]
        for c in range(4):
            pvn[:, b + 176 + c * 3:b + 176 + (c + 1) * 3] = ccw[:, c * 128:(c + 1) * 128].T
        pvn[:, b + 188:b + 192] = g("b_rg")[l][None, :]
        pvn[:, b + 192:b + 208] = g("b_re")[l][None, :]
    pvn[:, 2 * NPL:2 * NPL + 8] = _cols(g("norm_f_g"))
    pvn[:, 2 * NPL + 8] = EPS
    c0 = 2 * NPL + 9
    pidx = np.arange(128, dtype=np.float32)
    pvn[:, c0] = pidx
    pvn[:, c0 + 1:c0 + 5] = np.array([0.0, 512.0, 1024.0, 1536.0], np.float32)[None, :]
    pvn[:, c0 + 5:c0 + 12] = np.arange(7, dtype=np.float32)[None, :]
    for j in range(4):
        pvn[:, c0 + 12 + j] = j * 128 + pidx
        pvn[:, c0 + 16 + j] = j * 256 + pidx
    cst = np.zeros((128, 128 + 2048 + 128), np.float32)
    cst[:, 128 + 2048:] = (pidx[:, None] < pidx[None, :]).astype(np.float32)
    cst[:, 0:128] = np.eye(128, dtype=np.float32)
    for e in range(16):
        cst[e, 128 + e * 128:128 + (e + 1) * 128] = 1.0
    shared = {
        "pv": pvn, "cst": cst,
        "w_in": g("w_in"), "w_a_out": g("w_a_out"), "w_pool": g("w_pool_grp"), "w_c_out": g("w_c_out"),
        "w_o": g("w_o"), "w_xq": g("w_xq"), "w_xkv": g("w_xkv"), "w_xo": g("w_xo"),
        "w_r": np.ascontiguousarray(np.concatenate([g("w_rg"), g("w_re")], axis=-1)),
    }
    shared["w_eg"] = g("w_e_gate")
    shared["w_eu"] = g("w_e_up")
    shared["w_ed"] = np.ascontiguousarray(
        g("w_e_down").reshape(L, NE, 2, 128, D).transpose(0, 1, 3, 2, 4)).reshape(L, NE, 128, 2 * D)
    return shared


_CACHE = {}


def kernel(**inputs):
    x = np.asarray(inputs["x"], np.float32)
    mem = np.asarray(inputs["mem"], np.float32)
    shared = make_shared(inputs)
    if "nc" not in _CACHE:
        _CACHE["nc"] = build()[0]
    nc = _CACHE["nc"]
    B = x.shape[0]
    in_maps = []
    for b in range(B):
        m = dict(shared)
        m["xT"] = np.ascontiguousarray(x[b].T)
        m["memT"] = np.ascontiguousarray(mem[b].T)
        in_maps.append(m)
    res = run_bass_kernel_spmd(nc, in_maps, core_ids=list(range(B)))
    out = np.stack([np.ascontiguousarray(res.results[b]["outT"].T) for b in range(B)], axis=0)
    return out.astype(np.float32)
```

```python
import numpy as np
import concourse.bass as bass
import concourse.mybir as mybir
from concourse.bass_utils import run_bass_kernel_spmd

F32 = mybir.dt.float32
BF16 = mybir.dt.bfloat16
AF = mybir.ActivationFunctionType
ALU = mybir.AluOpType

L = 2
D = 1024
T = 2048
NT = 4
TW = 512
NMEM = 256
DIN = 6144
NE = 16
EPS = 1e-6
NPL = 208
NPV = 2 * NPL + 29
NU = 7
NSLOT = NU * 512
HW = 1040
I32 = mybir.dt.int32
ENGS = ("pe", "act", "dve", "pool", "sp")


class Tile:
    __slots__ = ("w", "r", "name")

    def __init__(self, name="", base=None):
        self.name = name
        self.w = dict(base.w) if base is not None else {}
        self.r = dict(base.r) if base is not None else {}


def merged_base(tiles):
    b = Tile("base")
    for t in tiles:
        for k, v in t.w.items():
            if b.w.get(k, -1) < v:
                b.w[k] = v
        for k, v in t.r.items():
            if b.r.get(k, -1) < v:
                b.r[k] = v
    for k, v in b.r.items():
        if b.w.get(k, -1) < v:
            b.w[k] = v
    return b


class Op:
    __slots__ = ("fn", "waits", "dma", "idx")


class Planner:
    def __init__(self, nc):
        self.nc = nc
        self.ops = {e: [] for e in ENGS}
        self.seen = {e: {} for e in ENGS}
        self.clock = {}
        self.dcum = {}
        self.eng = {"pe": nc.tensor, "act": nc.scalar, "dve": nc.vector, "pool": nc.gpsimd, "sp": nc.sync}

    def op(self, eng, fn, reads=(), writes=(), dma=None, after=()):
        own = ("eng", eng)
        deps = {}
        for t in after:
            for k, v in t.w.items():
                if deps.get(k, -1) < v:
                    deps[k] = v
        for t in reads:
            for k, v in t.w.items():
                if deps.get(k, -1) < v:
                    deps[k] = v
        for t in writes:
            for k, v in t.w.items():
                if deps.get(k, -1) < v:
                    deps[k] = v
            for k, v in t.r.items():
                if deps.get(k, -1) < v:
                    deps[k] = v
        seen = self.seen[eng]
        waits = []
        for k, v in deps.items():
            if k == own and eng == "pe":
                continue
            if seen.get(k, -1) >= v:
                continue
            waits.append((k, v))
        for k, v in waits:
            for k2, v2 in self.clock[(k, v)].items():
                if seen.get(k2, -1) < v2:
                    seen[k2] = v2
            if seen.get(k, -1) < v:
                seen[k] = v
        o = Op()
        o.fn = fn
        o.waits = waits
        o.dma = dma
        o.idx = len(self.ops[eng])
        self.ops[eng].append(o)
        if dma is None:
            ev = (own, o.idx)
        else:
            self.dcum[dma] = self.dcum.get(dma, 0) + 16
            ev = (("dma", dma), self.dcum[dma])
        self.clock[ev] = dict(seen)
        for t in reads:
            if t.r.get(ev[0], -1) < ev[1]:
                t.r[ev[0]] = ev[1]
        for t in writes:
            t.w = {ev[0]: ev[1]}
            t.r = {}
        return ev

    def emit(self, final_waits):
        nc = self.nc
        need = {e: set() for e in ENGS}
        for e in ENGS:
            for o in self.ops[e]:
                for k, v in o.waits:
                    if k[0] == "eng":
                        need[k[1]].add(v)
        for k, v in final_waits:
            if k[0] == "eng":
                need[k[1]].add(v)
        EPOCH = 1000
        rank = {e: {idx: i for i, idx in enumerate(sorted(need[e]))} for e in ENGS}
        esem = {e: [nc.alloc_semaphore("es_%s_%d" % (e, j)) for j in range(len(need[e]) // EPOCH + 1)] for e in ENGS}
        dsem = {d: nc.alloc_semaphore("ds_" + d) for d in self.dcum}

        def dowait(engobj, k, v):
            if k[0] == "eng":
                r = rank[k[1]][v]
                engobj.wait_ge(esem[k[1]][r // EPOCH], r % EPOCH + 1)
            else:
                engobj.wait_ge(dsem[k[1]], v)

        for e in ENGS:
            engobj = self.eng[e]
            for o in self.ops[e]:
                for k, v in o.waits:
                    dowait(engobj, k, v)
                ins = o.fn()
                if o.dma is not None:
                    ins.then_inc(dsem[o.dma], 16)
                elif o.idx in need[e]:
                    ins.then_inc(esem[e][rank[e][o.idx] // EPOCH], 1)
        for k, v in final_waits:
            dowait(nc.sync, k, v)
        self.stats = {e: len(self.ops[e]) for e in ENGS}


def build(stage=None, n_layers=L, layers=None, skip=()):
    nc = bass.Bass("TRN2", target_bir_lowering=False)
    dt = nc.dram_tensor
    xT = dt("xT", [D, T], F32, kind="ExternalInput").ap()
    memT = dt("memT", [D, NMEM], F32, kind="ExternalInput").ap()
    pv = dt("pv", [128, NPV], F32, kind="ExternalInput").ap()
    cst = dt("cst", [128, 128 + 2048 + 128], F32, kind="ExternalInput").ap()
    w_in = dt("w_in", [L, D, DIN], F32, kind="ExternalInput").ap()
    w_a_out = dt("w_a_out", [L, 512, D], F32, kind="ExternalInput").ap()
    w_pool = dt("w_pool", [L, 4, 128, 256], F32, kind="ExternalInput").ap()
    w_c_out = dt("w_c_out", [L, 512, D], F32, kind="ExternalInput").ap()
    w_o = dt("w_o", [L, D, D], F32, kind="ExternalInput").ap()
    w_xq = dt("w_xq", [L, D, D], F32, kind="ExternalInput").ap()
    w_xkv = dt("w_xkv", [L, D, 2 * D], F32, kind="ExternalInput").ap()
    w_xo = dt("w_xo", [L, D, D], F32, kind="ExternalInput").ap()
    w_r = dt("w_r", [L, D, 20], F32, kind="ExternalInput").ap()
    w_eg = dt("w_eg", [L, NE, D, 256], F32, kind="ExternalInput").ap()
    w_eu = dt("w_eu", [L, NE, D, 256], F32, kind="ExternalInput").ap()
    w_ed = dt("w_ed", [L, NE, 128, 2 * D], F32, kind="ExternalInput").ap()
    outT = dt("outT", [D, T], F32, kind="ExternalOutput").ap()
    hs_all = dt("hs", [L * NSLOT, HW], BF16, kind="Internal").ap()
    wbf_g = dt("wbf_g", [L * NE * 128, 2048], BF16, kind="Internal").ap()
    wbf_u = dt("wbf_u", [L * NE * 128, 2048], BF16, kind="Internal").ap()
    wbf_d = dt("wbf_d", [L * NE * 128, 2048], BF16, kind="Internal").ap()
    ys_d = dt("ys", [NSLOT, D], F32, kind="Internal").ap()

    P = Planner(nc)
    op = P.op

    def sb(name, nf32):
        return nc.alloc_sbuf_tensor(name, [128, nf32], F32)[:]

    RX = sb("RX", 16384)
    RH = sb("RH", 8192)
    RMG = sb("RMG", 8192)
    RBR = sb("RBR", 4096)
    RW = [sb("RW%d" % i, 2048) for i in range(4)]
    RS = sb("RS", 4160)
    PV = sb("PV", NPV)
    RSQ = sb("RSQ", 512)
    RRS = sb("RRS", 1024)
    RSG = sb("RSG", 1024)
    RC = sb("RC", 128)

    PSB = [nc.alloc_psum_tensor("ps%d" % i, [128, 512], F32)[:] for i in range(8)]
    PSt = [Tile("ps%d" % i) for i in range(8)]
    psctr = [0]

    def ps():
        i = psctr[0] % 8
        psctr[0] += 1
        return PSt[i], PSB[i]

    def bfv(ap):
        return ap.bitcast(BF16)

    def tsl(tt):
        return slice(tt * TW, (tt + 1) * TW)

    X = [RX[:, c * T:(c + 1) * T] for c in range(8)]
    Xt = [[Tile("x%d_%d" % (c, t)) for t in range(NT)] for c in range(8)]
    HB = bfv(RH)
    H = [HB[:, c * T:(c + 1) * T] for c in range(8)]
    MGB = bfv(RMG)
    MG = [MGB[:, c * T:(c + 1) * T] for c in range(8)]
    ACONV = [RMG[:, c * T:(c + 1) * T] for c in range(4)]
    BRB = bfv(RBR)
    BR4 = [BRB[:, c * T:(c + 1) * T] for c in range(4)]
    SQ = [bfv(RSQ[:, i * 256:(i + 1) * 256]) for i in range(2)]
    SQt = [Tile("sq%d" % i) for i in range(2)]
    RSTD = [RRS[:, i * 512:(i + 1) * 512] for i in range(2)]
    RSTDt = [Tile("rstd%d" % i) for i in range(2)]
    SG = [RSG[:, i * 512:(i + 1) * 512] for i in range(2)]
    SGt = [Tile("sg%d" % i) for i in range(2)]
    ONES = bfv(RC[:, 0:64])
    IDENT = bfv(RC[:, 64:128])
    Ct = Tile("consts")
    PVt = Tile("pv")
    ctr = {"sq": 0, "rs": 0, "sg": 0}

    def nxt(kind, n=2):
        i = ctr[kind] % n
        ctr[kind] += 1
        return i

    op("sp", lambda: nc.sync.dma_start(out=PV, in_=pv), writes=[PVt], dma="pv")
    for tt in range(NT):
        for c in range(8):
            ev_ = op("sp", (lambda c=c, tt=tt: nc.sync.dma_start(out=X[c][:, tsl(tt)], in_=xT[c * 128:(c + 1) * 128, tsl(tt)])),
                     writes=[Xt[c][tt]], dma="x%d" % tt)
        for c in range(8):
            Xt[c][tt].w = {ev_[0]: ev_[1]}
    op("dve", lambda: nc.vector.memset(ONES, 1.0), writes=[Ct])
    op("pool", lambda: nc.gpsimd.dma_start(out=IDENT, in_=cst[:, 0:128]), writes=[Ct], dma="cst")

    ZT = Tile("zsrc")
    ZSRC = bfv(RMG)[:, 0:HW]
    op("dve", lambda: nc.vector.memset(ZSRC, 0.0), writes=[ZT])
    HSt = [[Tile("hs%d_%d" % (l_, i)) for i in range(8)] for l_ in range(L)]
    for l_ in range(L):
        for a in range(NSLOT // 128):
            op("sp", (lambda l_=l_, a=a: nc.sync.dma_start(out=hs_all[l_ * NSLOT + a * 128:l_ * NSLOT + (a + 1) * 128, :], in_=ZSRC)),
               reads=[ZT], after=([HSt[l_ - 1][a % 2]] if l_ > 0 else []), writes=[HSt[l_][a % 2]], dma="z%d" % (a % 2))
    WZt = Tile("wz")
    for wb_ in (wbf_g, wbf_u, wbf_d):
        for a in range(NE, L * NE):
            for h_ in range(2):
                op("sp", (lambda wb_=wb_, a=a, h_=h_: nc.sync.dma_start(
                    out=wb_[a * 128:(a + 1) * 128, h_ * 1024:(h_ + 1) * 1024], in_=hs_all[0:128, 0:1024])),
                   reads=[HSt[0][0]], writes=[WZt], dma="wz%d" % h_)
    YSt = [Tile("ys%d" % i) for i in range(2)]
    UT = bfv(RS[:, 4052:4116])
    UTt = Tile("ut")
    op("pool", lambda: nc.gpsimd.dma_start(out=UT, in_=cst[:, 128 + 2048:128 + 2048 + 128]), writes=[UTt], dma="ut")

    Wt = [Tile("w%d" % i) for i in range(4)]
    wctr = [0]

    def wslot():
        i = wctr[0] % 4
        wctr[0] += 1
        return i

    def wview(i, k, n):
        return bfv(RW[i])[:, 0:k * n].rearrange("p (k n) -> p k n", k=k)

    PCt = [[] for _ in range(L)]
    bgq = []
    for l_ in range(L):
        for e in range(NE):
            r0 = (l_ * NE + e) * 128
            for dstT, srcv in ((wbf_g, w_eg[l_, e].rearrange("(p k) n -> p (k n)", k=8)),
                               (wbf_u, w_eu[l_, e].rearrange("(p k) n -> p (k n)", k=8)),
                               (wbf_d, w_ed[l_, e])):
                bgq.append((l_, dstT[r0:r0 + 128, :], srcv))

    def bg_issue(n, upto_layer=None, first_reads=()):
        rd = list(first_reads)
        while bgq and n > 0:
            if upto_layer is not None and bgq[0][0] > upto_layer:
                break
            l_, dst_, src_ = bgq.pop(0)
            t_ = Tile("pc")
            op("pool", (lambda dst_=dst_, src_=src_: nc.gpsimd.dma_start(out=dst_, in_=src_)),
               after=rd + ([WZt] if l_ > 0 else []), writes=[t_], dma="pc%d" % l_)
            rd = []
            PCt[l_].append(t_)
            n -= 1

    bg_rate = [0]

    def wload(i, dst, src):
        op("pool", lambda: nc.gpsimd.dma_start(out=dst, in_=src), writes=[Wt[i]], dma="w%d" % i)
        bg_issue(bg_rate[0])

    def load_kn(wap, c0, n, k=8, slot=None):
        i = wslot() if slot is None else slot
        v = wview(i, k, n)
        wload(i, v, wap.rearrange("(k p) n -> p k n", p=128)[:, :, c0:c0 + n])
        return i, v

    def mm(pst, psa, lhsT, rhs, start, stop, reads):
        op("pe", lambda: nc.tensor.matmul(psa, lhsT, rhs, start=start, stop=stop), reads=reads, writes=[pst])

    def pcol(col):
        return PV[:, col:col + 1]

    EPSC = pcol(2 * NPL + 8)

    def rms_stats(srcs, srct, width, tt_slices):
        res = []
        for (sl, tts) in tt_slices:
            pt, pa = ps()
            pa = pa[:, 0:width]
            for c in range(8):
                i = nxt("sq")
                sq = SQ[i][:, 0:width]
                op("act", (lambda sq=sq, c=c, sl=sl: nc.scalar.activation(sq, srcs[c][:, sl], AF.Square)),
                   reads=[srct[c][tts]], writes=[SQt[i]])
                mm(pt, pa, ONES, sq, c == 0, c == 7, [SQt[i], Ct])
            j = nxt("rs")
            rs = RSTD[j][:, 0:width]
            op("act", (lambda rs=rs, pa=pa: nc.scalar.activation(rs, pa, AF.Ln, bias=EPSC, scale=1.0 / D)),
               reads=[pt, PVt], writes=[RSTDt[j]])
            op("act", (lambda rs=rs: nc.scalar.activation(rs, rs, AF.Exp, scale=-0.5)),
               reads=[RSTDt[j]], writes=[RSTDt[j]])
            res.append((RSTDt[j], rs))
        return res

    def norm_tile(tt, gcol, Ht):
        (rt, rs), = rms_stats(X, Xt, TW, [(tsl(tt), tt)])
        for c in range(8):
            op("dve", (lambda c=c, tt=tt, rs=rs: nc.vector.scalar_tensor_tensor(
                out=H[c][:, tsl(tt)], in0=X[c][:, tsl(tt)], scalar=pcol(gcol + c), in1=rs,
                op0=ALU.mult, op1=ALU.mult)),
               reads=[Xt[c][tt], rt, PVt], writes=[Ht[c][tt]])

    def rmsnorm_to_H(gcol, Ht):
        for tt in range(NT):
            norm_tile(tt, gcol, Ht)

    def new_tiles(name, n, m, base):
        return [[Tile("%s%d_%d" % (name, c, t), base) for t in range(m)] for c in range(n)]

    def flat(tl):
        return [t for row in tl for t in row]

    hbase = Tile("hb")
    Ht_pre = None
    final = []
    oi = [0]
    mgbase = merged_base([ZT])
    brbase = Tile("brb")
    sbase = Tile("sb")

    OB = [RS[:, i * 512:(i + 1) * 512] for i in range(3)]
    OBt = [None, None, None]

    def final_tile(tt):
        (rt, rs), = rms_stats(X, Xt, TW, [(tsl(tt), tt)])
        for c in range(8):
            i = oi[0] % 3
            oi[0] += 1
            if OBt[i] is None:
                OBt[i] = Tile("ob%d" % i, obbase[0])
            op("dve", (lambda c=c, tt=tt, rs=rs, i=i: nc.vector.scalar_tensor_tensor(
                out=OB[i], in0=X[c][:, tsl(tt)], scalar=pcol(2 * NPL + c), in1=rs, op0=ALU.mult, op1=ALU.mult)),
               reads=[Xt[c][tt], rt, PVt], writes=[OBt[i]])
            ev = op("sp", (lambda c=c, tt=tt, i=i: nc.sync.dma_start(out=outT[c * 128:(c + 1) * 128, tsl(tt)], in_=OB[i])),
                    reads=[OBt[i]], dma="o%d" % i)
            final.append(ev)

    obbase = [None]
    pre_w = [None]
    for l in (layers if layers is not None else range(n_layers)):
        pb = l * NPL
        if Ht_pre is None:
            Ht = new_tiles("h", 8, NT, hbase)
            rmsnorm_to_H(pb + 0, Ht)
        else:
            Ht = Ht_pre

        ACt = new_tiles("aconv", 4, NT, mgbase)
        APAD = [bfv(RS[:, i * 1040:i * 1040 + 1039]) for i in range(2)]
        APt = [[Tile("apad%d_%d" % (i, t), sbase) for t in range(NT + 1)] for i in range(2)]
        DG = [BRB[:, i * 3968:(i + 1) * 3968].rearrange("p (k n) -> p k n", k=31) for i in range(2)]
        DGt = [[Tile("dg%d_%d" % (i, k), brbase) for k in range(31)] for i in range(2)]
        for i in range(2):
            op("dve", (lambda i=i: nc.vector.memset(APAD[i][:, 0:30], 0.0)), writes=[APt[i][0]])
        if pre_w[0] is not None:
            (sv, Wv), (sg_, Wg) = pre_w[0]
            pre_w[0] = None
        else:
            sv, Wv = load_kn(w_in[l], 0, 512)
            sg_, Wg = load_kn(w_in[l], 512, 512)

        DGc = {0: (DG[0], DGt[0], None), 1: (DG[1], DGt[1], None)}

        def glu(c):
            ap_i = c % 2
            wc = pb + 40 + c * 31
            if c >= 2:
                si = wslot()
                DGc[c] = (wview(si, 31, 128), [Tile("dgr%d_%d" % (c, k), Wt[si]) for k in range(31)], si)
            dgv, dgt, _ = DGc[c]
            for k in range(31):
                op("act", (lambda dgv=dgv, k=k, wc=wc: nc.scalar.activation(dgv[:, k, :], IDENT, AF.Copy, scale=pcol(wc + k))),
                   reads=[Ct, PVt], writes=[dgt[k]])
            for tt in range(NT):
                pvt, pva = ps()
                for k in range(8):
                    mm(pvt, pva, Wv[:, k, c * 128:(c + 1) * 128], H[k][:, tsl(tt)], k == 0, k == 7, [Wt[sv], Ht[k][tt]])
                pgt, pga = ps()
                for k in range(8):
                    mm(pgt, pga, Wg[:, k, c * 128:(c + 1) * 128], H[k][:, tsl(tt)], k == 0, k == 7, [Wt[sg_], Ht[k][tt]])
                j = nxt("sg")
                op("act", (lambda j=j, pga=pga: nc.scalar.activation(SG[j], pga, AF.Sigmoid)),
                   reads=[pgt], writes=[SGt[j]])
                op("dve", (lambda j=j, pva=pva, ap_i=ap_i, tt=tt: nc.vector.tensor_tensor(
                    APAD[ap_i][:, 30 + tt * TW:30 + (tt + 1) * TW], pva, SG[j], op=ALU.mult)),
                   reads=[pvt, SGt[j]], writes=[APt[ap_i][tt + 1]])

        def conv(c, tts=range(NT)):
            ap_i = c % 2
            dgv, dgt, _ = DGc[c]
            for tt in tts:
                pt, pa = ps()
                for k in range(31):
                    mm(pt, pa, dgv[:, k, :], APAD[ap_i][:, tt * TW + k:tt * TW + k + TW], k == 0, k == 30,
                       [dgt[k], APt[ap_i][tt], APt[ap_i][tt + 1]])
                op("act", (lambda c=c, tt=tt, pa=pa, pb=pb: nc.scalar.activation(
                    ACONV[c][:, tsl(tt)], pa, AF.Identity, bias=pcol(pb + 164 + c))),
                   reads=[pt, PVt], writes=[ACt[c][tt]])

        AACT = BR4

        def ln(tt):
            p1t, p1a = ps()
            p2t, p2a = ps()
            for c in range(4):
                i = nxt("sq")
                op("act", (lambda i=i, c=c, tt=tt: nc.scalar.activation(SQ[i], ACONV[c][:, tsl(tt)], AF.Copy)),
                   reads=[ACt[c][tt]], writes=[SQt[i]])
                mm(p1t, p1a, ONES, SQ[i], c == 0, c == 3, [SQt[i], Ct])
                i2 = nxt("sq")
                op("act", (lambda i2=i2, c=c, tt=tt: nc.scalar.activation(SQ[i2], ACONV[c][:, tsl(tt)], AF.Square)),
                   reads=[ACt[c][tt]], writes=[SQt[i2]])
                mm(p2t, p2a, ONES, SQ[i2], c == 0, c == 3, [SQt[i2], Ct])
            jm = nxt("rs")
            mean = RSTD[jm]
            op("dve", (lambda mean=mean, p1a=p1a: nc.vector.tensor_single_scalar(mean, p1a, 1.0 / 512, op=ALU.mult)),
               reads=[p1t], writes=[RSTDt[jm]])
            jv = nxt("sg")
            var = SG[jv]
            op("dve", (lambda var=var, mean=mean: nc.vector.tensor_tensor(var, mean, mean, op=ALU.mult)),
               reads=[RSTDt[jm]], writes=[SGt[jv]])
            op("dve", (lambda var=var, p2a=p2a: nc.vector.scalar_tensor_tensor(
                out=var, in0=p2a, scalar=1.0 / 512, in1=var, op0=ALU.mult, op1=ALU.subtract)),
               reads=[p2t, SGt[jv]], writes=[SGt[jv]])
            op("act", (lambda var=var: nc.scalar.activation(var, var, AF.Ln, bias=EPSC, scale=1.0)),
               reads=[SGt[jv], PVt], writes=[SGt[jv]])
            op("act", (lambda var=var: nc.scalar.activation(var, var, AF.Exp, scale=-0.5)),
               reads=[SGt[jv]], writes=[SGt[jv]])
            for c in range(4):
                dst = ACONV[c][:, tsl(tt)]
                op("dve", (lambda dst=dst, mean=mean: nc.vector.tensor_tensor(dst, dst, mean, op=ALU.subtract)),
                   reads=[ACt[c][tt], RSTDt[jm]], writes=[ACt[c][tt]])
                op("dve", (lambda dst=dst, var=var: nc.vector.tensor_tensor(dst, dst, var, op=ALU.mult)),
                   reads=[ACt[c][tt], SGt[jv]], writes=[ACt[c][tt]])
                op("act", (lambda dst=dst, c=c, tt=tt, pb=pb: nc.scalar.activation(
                    AACT[c][:, tsl(tt)], dst, AF.Silu, bias=pcol(pb + 172 + c), scale=pcol(pb + 168 + c))),
                   reads=[ACt[c][tt], PVt], writes=[AAt[c][tt]])

        glu(0)
        glu(1)
        bg_issue(24, upto_layer=l, first_reads=[APt[0][NT]] + flat(HSt))
        conv(0)
        glu(2)
        conv(1)
        glu(3)
        brbase = merged_base(flat(DGt))
        AAt = new_tiles("aact", 4, NT, brbase)
        conv(2, [0])
        conv(3, [0])
        conv(2, [1])
        conv(3, [1])
        ln(0)
        conv(2, [2])
        conv(3, [2])
        ln(1)
        conv(2, [3])
        conv(3, [3])
        ln(2)
        ln(3)
        for c_ in (2, 3):
            _, taps_, si_ = DGc[c_]
            mb_ = merged_base(taps_)
            Wt[si_].w = mb_.w
            Wt[si_].r = {}
        sbase = merged_base(flat(APt))
        MGt = new_tiles("mg", 8, NT, merged_base(flat(ACt)))
        sa, WA = load_kn(w_a_out[l], 0, 1024, k=4)

        def gated_out(gate_c0, branch_fn, first):
            for half in range(2):
                sgt, WG = load_kn(w_in[l], gate_c0 + half * 512, 512)
                for o4 in range(4):
                    oc = half * 4 + o4
                    for tt in range(NT):
                        pbt, pba, scale_col = branch_fn(oc, tt)
                        pgt, pga = ps()
                        for k in range(8):
                            mm(pgt, pga, WG[:, k, o4 * 128:(o4 + 1) * 128], H[k][:, tsl(tt)], k == 0, k == 7,
                               [Wt[sgt], Ht[k][tt]])
                        j = nxt("sg")
                        op("act", (lambda j=j, pga=pga: nc.scalar.activation(SG[j], pga, AF.Sigmoid)),
                           reads=[pgt], writes=[SGt[j]])
                        dst = MG[oc][:, tsl(tt)]
                        if first:
                            op("dve", (lambda j=j, pba=pba, dst=dst: nc.vector.tensor_tensor(dst, pba, SG[j], op=ALU.mult)),
                               reads=[pbt, SGt[j]], writes=[MGt[oc][tt]])
                        else:
                            if scale_col is None:
                                op("dve", (lambda j=j, pba=pba: nc.vector.tensor_tensor(SG[j], pba, SG[j], op=ALU.mult)),
                                   reads=[pbt, SGt[j]], writes=[SGt[j]])
                            else:
                                op("dve", (lambda j=j, pba=pba, sc=scale_col: nc.vector.scalar_tensor_tensor(
                                    out=SG[j], in0=pba, scalar=pcol(sc), in1=SG[j], op0=ALU.mult, op1=ALU.mult)),
                                   reads=[pbt, SGt[j], PVt], writes=[SGt[j]])
                            op("dve", (lambda j=j, dst=dst: nc.vector.tensor_tensor(dst, dst, SG[j], op=ALU.add)),
                               reads=[MGt[oc][tt], SGt[j]], writes=[MGt[oc][tt]])

        def a_branch(oc, tt):
            pt, pa = ps()
            for k in range(4):
                mm(pt, pa, WA[:, k, oc * 128:(oc + 1) * 128], AACT[k][:, tsl(tt)], k == 0, k == 3, [Wt[sa], AAt[k][tt]])
            return pt, pa, None

        gated_out(3072, a_branch, True)
        brbase = merged_base(flat(AAt))

        PPt = new_tiles("pp", 4, NT, brbase)
        PP = BR4
        UPAD = RS[:, 0:16 + T]
        UPt = [Tile("upad%d" % t, sbase) for t in range(NT + 1)]
        LA = RS[:, 2080:2080 + 528]
        LB = RS[:, 2080 + 528:2080 + 1056]
        INVC = RS[:, 3200:3200 + 64]
        RCP = RS[:, 3264:3280]
        LAt = Tile("la", sbase)
        LBt = Tile("lb", sbase)
        IVt = Tile("invc", sbase)
        op("dve", lambda: nc.vector.memset(UPAD[:, 0:16], 0.0), writes=[UPt[0]])
        for jj in range(16):
            op("dve", (lambda jj=jj: nc.vector.memset(RCP[:, jj:jj + 1], 1.0 / (jj + 1))), writes=[IVt])
        for g, w in enumerate((2, 4, 8, 16)):
            op("dve", (lambda g=g, w=w: nc.vector.tensor_single_scalar(INVC[:, g * 16:(g + 1) * 16], RCP, 1.0 / w, op=ALU.max)),
               reads=[IVt], writes=[IVt])
        su, WU = load_kn(w_in[l], 1024, 512)
        for g, w in enumerate((2, 4, 8, 16)):
            nlev = g + 1
            for tt in range(NT):
                pt, pa = ps()
                for k in range(8):
                    mm(pt, pa, WU[:, k, g * 128:(g + 1) * 128], H[k][:, tsl(tt)], k == 0, k == 7, [Wt[su], Ht[k][tt]])
                op("act", (lambda pa=pa, tt=tt: nc.scalar.activation(UPAD[:, 16 + tt * TW:16 + (tt + 1) * TW], pa, AF.Copy)),
                   reads=[pt], writes=[UPt[tt + 1]])
                off = tt * TW
                bufs = [(LA, LAt), (LB, LBt)]
                srcb, srct_ = UPAD[:, off:off + 528], None
                rd = [UPt[tt], UPt[tt + 1]]
                for lev in range(nlev):
                    d = 1 << lev
                    dstb, dstt = bufs[lev % 2]
                    lo = 2 * d
                    op("dve", (lambda dstb=dstb, srcb=srcb, lo=lo, d=d: nc.vector.tensor_tensor(
                        dstb[:, lo:528], srcb[:, lo:528], srcb[:, lo - d:528 - d], op=ALU.add)),
                       reads=rd, writes=[dstt])
                    srcb, rd = dstb, [dstt]
                dst = PP[g][:, tsl(tt)]
                op("dve", (lambda dst=dst, srcb=srcb, w=w, off=off: nc.vector.scalar_tensor_tensor(
                    out=dst, in0=srcb[:, 16:528], scalar=1.0 / w, in1=UPAD[:, off + 16:off + 528],
                    op0=ALU.mult, op1=ALU.subtract)),
                   reads=rd + [UPt[tt + 1]], writes=[PPt[g][tt]])
                if tt == 0:
                    op("dve", (lambda srcb=srcb, g=g: nc.vector.tensor_tensor(
                        srcb[:, 16:32], srcb[:, 16:32], INVC[:, g * 16:(g + 1) * 16], op=ALU.mult)),
                       reads=rd + [IVt], writes=[rd[0]])
                    op("dve", (lambda dst=dst, srcb=srcb: nc.vector.tensor_tensor(
                        dst[:, 0:16], srcb[:, 16:32], UPAD[:, 16:32], op=ALU.subtract)),
                       reads=rd + [UPt[1]], writes=[PPt[g][tt]])
        sbase = merged_base(UPt + [LAt, LBt, IVt])
        sp_i = wslot()
        WP = wview(sp_i, 4, 256)
        wload(sp_i, WP, w_pool[l].rearrange("g p n -> p g n"))

        def b_branch(oc, tt):
            pt, pa = ps()
            g, o2 = oc // 2, oc % 2
            mm(pt, pa, WP[:, g, o2 * 128:(o2 + 1) * 128], PP[g][:, tsl(tt)], True, True, [Wt[sp_i], PPt[g][tt]])
            return pt, pa, pb + 32 + oc

        gated_out(4096, b_branch, False)
        brbase = merged_base(flat(PPt))

        CCt = new_tiles("cc", 4, NT, brbase)
        CC = BR4
        CPAD = RS[:, 0:2 + T]
        CPt = [Tile("cpad%d" % t, sbase) for t in range(NT + 1)]
        CTMP = [RS[:, 2080 + i * 512:2080 + (i + 1) * 512] for i in range(2)]
        CTt = [Tile("ctmp%d" % i, sbase) for i in range(2)]
        op("dve", lambda: nc.vector.memset(CPAD[:, 0:2], 0.0), writes=[CPt[0]])
        sx, WX = load_kn(w_in[l], 1536, 512)
        sc_, WCc = load_kn(w_in[l], 2560, 512)
        sb_, WB = load_kn(w_in[l], 2048, 512)
        for c in range(4):
            for tt in range(NT):
                pxt, pxa = ps()
                for k in range(8):
                    mm(pxt, pxa, WX[:, k, c * 128:(c + 1) * 128], H[k][:, tsl(tt)], k == 0, k == 7, [Wt[sx], Ht[k][tt]])
                pct, pca = ps()
                for k in range(8):
                    mm(pct, pca, WCc[:, k, c * 128:(c + 1) * 128], H[k][:, tsl(tt)], k == 0, k == 7, [Wt[sc_], Ht[k][tt]])
                j = nxt("sg")
                op("act", (lambda j=j, pxa=pxa: nc.scalar.activation(SG[j], pxa, AF.Copy)), reads=[pxt], writes=[SGt[j]])
                op("dve", (lambda j=j, pca=pca, tt=tt: nc.vector.tensor_tensor(
                    CPAD[:, 2 + tt * TW:2 + (tt + 1) * TW], pca, SG[j], op=ALU.mult)),
                   reads=[pct, SGt[j]], writes=[CPt[tt + 1]])
                pbt, pba = ps()
                for k in range(8):
                    mm(pbt, pba, WB[:, k, c * 128:(c + 1) * 128], H[k][:, tsl(tt)], k == 0, k == 7, [Wt[sb_], Ht[k][tt]])
                ci = (c * NT + tt) % 2
                wc = pb + 176 + c * 3
                rd = [CPt[tt], CPt[tt + 1], PVt]
                op("dve", (lambda ci=ci, tt=tt, wc=wc: nc.vector.tensor_single_scalar(
                    CTMP[ci], CPAD[:, tt * TW:tt * TW + TW], pcol(wc), op=ALU.mult)),
                   reads=rd, writes=[CTt[ci]])
                for k in (1, 2):
                    op("dve", (lambda ci=ci, tt=tt, wc=wc, k=k: nc.vector.scalar_tensor_tensor(
                        out=CTMP[ci], in0=CPAD[:, tt * TW + k:tt * TW + k + TW], scalar=pcol(wc + k), in1=CTMP[ci],
                        op0=ALU.mult, op1=ALU.add)),
                       reads=rd + [CTt[ci]], writes=[CTt[ci]])
                op("dve", (lambda ci=ci, c=c, tt=tt, pba=pba: nc.vector.tensor_tensor(
                    CC[c][:, tsl(tt)], pba, CTMP[ci], op=ALU.mult)),
                   reads=[pbt, CTt[ci]], writes=[CCt[c][tt]])
        sbase = merged_base(CPt + CTt)
        sco, WCO = load_kn(w_c_out[l], 0, 1024, k=4)

        def c_branch(oc, tt):
            pt, pa = ps()
            for k in range(4):
                mm(pt, pa, WCO[:, k, oc * 128:(oc + 1) * 128], CC[k][:, tsl(tt)], k == 0, k == 3, [Wt[sco], CCt[k][tt]])
            return pt, pa, None

        gated_out(5120, c_branch, False)
        brbase = merged_base(flat(CCt))
        hbase = merged_base(flat(Ht))

        def proj_add(wap, SRC, SRCt, gcol, base_fn, after_loads=None, Wh=None):
            if Wh is None:
                Wh = [load_kn(wap, half * 512, 512) for half in range(2)]
            if after_loads is not None:
                after_loads()
            Hn = [[None] * NT for _ in range(8)]
            for tt in range(NT):
                for oc in range(8):
                    s_, Wv_ = Wh[oc // 4]
                    o4 = oc % 4
                    pt, pa = ps()
                    for k in range(8):
                        mm(pt, pa, Wv_[:, k, o4 * 128:(o4 + 1) * 128], SRC[k][:, tsl(tt)], k == 0, k == 7,
                           [Wt[s_], SRCt[k][tt]])
                    dst = X[oc][:, tsl(tt)]
                    op("dve", (lambda dst=dst, pa=pa: nc.vector.tensor_tensor(dst, dst, pa, op=ALU.add)),
                       reads=[pt, Xt[oc][tt]], writes=[Xt[oc][tt]])
                for c in range(8):
                    Hn[c][tt] = Tile("h%d_%d" % (c, tt), base_fn(c, tt))
                norm_tile(tt, gcol, Hn)
            return Hn

        MEMT = RS[:, 0:2048].rearrange("p (k n) -> p k n", k=8)
        MEMTt = Tile("memt", sbase)
        op("sp", lambda: nc.sync.dma_start(out=MEMT, in_=memT.rearrange("(k p) n -> p k n", p=128)),
           writes=[MEMTt], dma="mem")
        MEMN = BRB[:, 4096:6144].rearrange("p (k n) -> p k n", k=8)
        MEMNt = Tile("memn", brbase)
        KT = BRB[:, 0:2048].rearrange("p (k n) -> p k n", k=8)
        KTt = Tile("kt", brbase)
        VV = BRB[:, 2048:4096].rearrange("p (k n) -> p k n", k=2)
        VVt = Tile("vv", brbase)
        memsrc = [MEMT[:, c, :] for c in range(8)]
        (rt, rs), = rms_stats(memsrc, [[MEMTt]] * 8, NMEM, [(slice(0, NMEM), 0)])
        for c in range(8):
            op("dve", (lambda c=c, rs=rs, pb=pb: nc.vector.scalar_tensor_tensor(
                out=MEMN[:, c, :], in0=MEMT[:, c, :], scalar=pcol(pb + 24 + c), in1=rs, op0=ALU.mult, op1=ALU.mult)),
               reads=[MEMTt, rt, PVt], writes=[MEMNt])
        for half in range(2):
            s_, Wk = load_kn(w_xkv[l], half * 512, 512)
            for o4 in range(4):
                dc = half * 4 + o4
                pt, pa = ps()
                pa = pa[:, 0:NMEM]
                for k in range(8):
                    mm(pt, pa, Wk[:, k, o4 * 128:(o4 + 1) * 128], MEMN[:, k, :], k == 0, k == 7, [Wt[s_], MEMNt])
                op("act", (lambda dc=dc, pa=pa: nc.scalar.activation(KT[:, dc, :], pa, AF.Copy)), reads=[pt], writes=[KTt])
        for half in range(2):
            s_, Wvv = load_kn(w_xkv[l], 1024 + half * 512, 512)
            for mc in range(2):
                pt, pa = ps()
                for k in range(8):
                    mm(pt, pa, MEMN[:, k, mc * 128:(mc + 1) * 128], Wvv[:, k, :], k == 0, k == 7, [Wt[s_], MEMNt])
                op("act", (lambda mc=mc, half=half, pa=pa: nc.scalar.activation(
                    VV[:, mc, half * 512:(half + 1) * 512], pa, AF.Copy)), reads=[pt], writes=[VVt])
        sbase = merged_base([MEMTt])
        hb_ = hbase
        Ht = proj_add(w_o[l], MG, MGt, pb + 8, lambda c, tt: hb_)
        mgbase = merged_base(flat(MGt))
        if stage == ("mix", l):
            break
        if ("postmix", l) in skip:
            continue

        Qt = new_tiles("q", 8, NT, mgbase)
        Q = MG
        for half in range(2):
            s_, Wq = load_kn(w_xq[l], half * 512, 512)
            for o4 in range(4):
                dc = half * 4 + o4
                for tt in range(NT):
                    pt, pa = ps()
                    for k in range(8):
                        mm(pt, pa, Wq[:, k, o4 * 128:(o4 + 1) * 128], H[k][:, tsl(tt)], k == 0, k == 7, [Wt[s_], Ht[k][tt]])
                    op("act", (lambda dc=dc, tt=tt, pa=pa: nc.scalar.activation(Q[dc][:, tsl(tt)], pa, AF.Copy)),
                       reads=[pt], writes=[Qt[dc][tt]])
        Wh_xo = [load_kn(w_xo[l], half * 512, 512) for half in range(2)]
        bg_issue(1000, upto_layer=l)
        hbase = merged_base(flat(Ht))
        Ot = new_tiles("o", 8, NT, hbase)
        O = H
        EE = [bfv(RS[:, i * 512:(i + 1) * 512]).rearrange("p (k n) -> p k n", k=2) for i in range(3)]
        EEt = [[Tile("ee%d_%d" % (i, m), sbase) for m in range(2)] for i in range(3)]
        RD = [RS[:, 1536 + i * 512:1536 + (i + 1) * 512] for i in range(2)]
        RDt = [Tile("rd%d" % i, sbase) for i in range(2)]

        def att_S(u, tt, h):
            ei = u % 3
            for mc in range(2):
                pt, pa = ps()
                for j in range(2):
                    mm(pt, pa, KT[:, 2 * h + j, mc * 128:(mc + 1) * 128], Q[2 * h + j][:, tsl(tt)], j == 0, j == 1,
                       [KTt, Qt[2 * h + j][tt]])
                op("act", (lambda ei=ei, mc=mc, pa=pa: nc.scalar.activation(EE[ei][:, mc, :], pa, AF.Exp, scale=1.0 / 16)),
                   reads=[pt], writes=[EEt[ei][mc]])

        def att_D(u, tt, h):
            ei = u % 3
            ri = u % 2
            pdt, pda = ps()
            for mc in range(2):
                mm(pdt, pda, ONES, EE[ei][:, mc, :], mc == 0, mc == 1, [EEt[ei][mc], Ct])
            op("act", (lambda ri=ri, pda=pda: nc.scalar.activation(RD[ri], pda, AF.Ln)), reads=[pdt], writes=[RDt[ri]])
            op("act", (lambda ri=ri: nc.scalar.activation(RD[ri], RD[ri], AF.Exp, scale=-1.0)), reads=[RDt[ri]], writes=[RDt[ri]])
            for j in range(2):
                dc = 2 * h + j
                pt, pa = ps()
                for mc in range(2):
                    mm(pt, pa, VV[:, mc, dc * 128:(dc + 1) * 128], EE[ei][:, mc, :], mc == 0, mc == 1, [VVt, EEt[ei][mc]])
                op("dve", (lambda dc=dc, tt=tt, pa=pa, ri=ri: nc.vector.tensor_tensor(
                    O[dc][:, tsl(tt)], pa, RD[ri], op=ALU.mult)),
                   reads=[pt, RDt[ri]], writes=[Ot[dc][tt]])

        aunits = [(tt, h) for tt in range(NT) for h in range(4)]
        for u, (tt, h) in enumerate(aunits):
            att_S(u, tt, h)
            if u > 0:
                att_D(u - 1, *aunits[u - 1])
        att_D(len(aunits) - 1, *aunits[-1])
        sbase = merged_base(flat(EEt) + RDt)
        mgbase = merged_base(flat(Qt))
        brbase = merged_base([MEMNt, KTt, VVt])
        WGUb = [bfv(RMG[:, i * 2048:(i + 1) * 2048]) for i in range(4)]
        WGt = [Tile("wg%d" % i, mgbase) for i in range(4)]
        WUt = [Tile("wu%d" % i, mgbase) for i in range(4)]
        def prefetch_unit0(l=l):
            if not (stage is None or stage[0] not in ("mix", "xattn")):
                return
            bg_issue(1000, upto_layer=l)
            for j in range(4):
                r0 = (l * NE + j) * 128
                op("pool", (lambda j=j, r0=r0: nc.gpsimd.dma_start(out=WGUb[j][:, 0:2048], in_=wbf_g[r0:r0 + 128, :])),
                   reads=PCt[l], writes=[WGt[j]], dma="wg%d" % j)
                op("pool", (lambda j=j, r0=r0: nc.gpsimd.dma_start(out=WGUb[j][:, 2048:4096], in_=wbf_u[r0:r0 + 128, :])),
                   reads=PCt[l], writes=[WUt[j]], dma="wu%d" % j)
        Ht = proj_add(w_xo[l], O, Ot, pb + 16, lambda c, tt: Ot[c][tt], after_loads=prefetch_unit0, Wh=Wh_xo)
        if stage == ("xattn", l):
            break
        if ("moe", l) in skip:
            continue

        sr = wslot()
        WR = wview(sr, 8, 20)
        wload(sr, WR, w_r[l].rearrange("(k p) n -> p k n", p=128))
        AX = mybir.AxisListType.X
        rt_ = Tile("router", sbase)

        def rs_(a, n):
            return RS[:, a:a + n]
        LG = rs_(0, 320).rearrange("p (t j) -> p t j", j=20)
        Dm_f, ED_f, GS_f, LES_f, LES2_f, M1_f, M2_f, TM_f, GL_f = [rs_(320 + 64 * i, 64) for i in range(9)]
        v3 = lambda ap: ap.rearrange("p (t j) -> p t j", j=4)
        Dm, ED, GSEL, LES, LES2, M1, M2, TM, GL = [v3(x_) for x_ in (Dm_f, ED_f, GS_f, LES_f, LES2_f, M1_f, M2_f, TM_f, GL_f)]
        sm = [rs_(896 + 16 * i, 16) for i in range(12)]
        GG = rs_(1088, 256)
        GG4 = GG.rearrange("p (t j i) -> p t j i", j=4, i=4)
        GD = rs_(1344, 256)
        GHI = bfv(rs_(1600, 128))
        GLO = bfv(rs_(1728, 128))
        LP = rs_(1344, 256).rearrange("p (t j i) -> p t j i", j=4, i=4)

        def dv(fn, rd=()):
            op("dve", fn, reads=[rt_] + list(rd), writes=[rt_])

        def bc3(ap):
            return ap.unsqueeze(2).to_broadcast([128, 16, 4])
        TT_ = nc.vector.tensor_tensor
        RED = nc.vector.tensor_reduce
        plt, pla = ps()
        for tc in range(16):
            for k in range(8):
                mm(plt, pla[:, tc * 20:(tc + 1) * 20], H[k][:, tc * 128:(tc + 1) * 128], WR[:, k, :], k == 0, k == 7,
                   [Wt[sr], Ht[k][tc // 4]])
        ROW = ([bfv(RBR[:, 2048 + i * 520:2048 + (i + 1) * 520]) for i in range(2)]
               + [bfv(RBR[:, i * 520:(i + 1) * 520]) for i in range(3)]
               + [bfv(RW[3][:, i * 520:(i + 1) * 520]) for i in range(3)])
        ROWK = [r_[:, 0:1024].rearrange("p (j k) -> p k j", k=8) for r_ in ROW]
        ROWt = ([Tile("row%d" % i, brbase) for i in range(5)] + [Tile("row%d" % (5 + i), Wt[3]) for i in range(3)])

        def stage_rows(tc):
            i = tc % 8
            pt, pa = ps()
            pbf = pa.bitcast(BF16)
            for k in range(8):
                op("pe", (lambda pbf=pbf, k=k, tc=tc: nc.tensor.transpose(
                    pbf[:, k * 128:(k + 1) * 128], H[k][:, tc * 128:(tc + 1) * 128], IDENT)),
                   reads=[Ht[k][tc // 4], Ct], writes=[pt])
            op("act", (lambda i=i, pbf=pbf: nc.scalar.activation(ROW[i][:, 0:1024], pbf, AF.Copy)), reads=[pt], writes=[ROWt[i]])

        dv((lambda pla=pla, pb=pb: TT_(LG, pla[:, 0:320].rearrange("p (t j) -> p t j", j=20),
                                        PV[:, pb + 188:pb + 208].unsqueeze(1).to_broadcast([128, 16, 20]), op=ALU.add)), [plt, PVt])
        for tc in range(8):
            stage_rows(tc)
        gmax, gden, gval, l1, l2, dd, ed, w1, w2, tmp = sm[:10]
        LE = LG[:, :, 4:20].rearrange("p t (j i) -> p t j i", i=4)
        dv(lambda: RED(out=gmax, in_=LG[:, :, 0:4], axis=AX, op=ALU.max))
        dv(lambda: TT_(Dm, LG[:, :, 0:4], bc3(gmax), op=ALU.subtract))
        op("act", lambda: nc.scalar.activation(ED_f, Dm_f, AF.Exp), reads=[rt_], writes=[rt_])
        dv(lambda: RED(out=gden, in_=ED, axis=AX, op=ALU.add))
        dv(lambda: nc.vector.reciprocal(gval, gden))
        dv(lambda: nc.vector.tensor_single_scalar(GS_f, Dm_f, 0.0, op=ALU.is_ge))
        dv(lambda: TT_(LP, LE, GSEL.unsqueeze(3).to_broadcast([128, 16, 4, 4]), op=ALU.mult))
        dv(lambda: RED(out=LES, in_=LP.rearrange("p t j i -> p t i j"), axis=AX, op=ALU.add))
        dv(lambda: RED(out=l1, in_=LES, axis=AX, op=ALU.max))
        dv(lambda: TT_(TM, LES, bc3(l1), op=ALU.subtract))
        dv(lambda: nc.vector.tensor_single_scalar(M1_f, TM_f, 0.0, op=ALU.is_ge))
        dv(lambda: nc.vector.scalar_tensor_tensor(out=LES2_f, in0=M1_f, scalar=-1e30, in1=LES_f, op0=ALU.mult, op1=ALU.add))
        dv(lambda: RED(out=l2, in_=LES2, axis=AX, op=ALU.max))
        dv(lambda: TT_(TM, LES2, bc3(l2), op=ALU.subtract))
        dv(lambda: nc.vector.tensor_single_scalar(M2_f, TM_f, 0.0, op=ALU.is_ge))
        dv(lambda: TT_(dd, l2, l1, op=ALU.subtract))
        op("act", lambda: nc.scalar.activation(ed, dd, AF.Exp), reads=[rt_], writes=[rt_])
        dv(lambda: nc.vector.tensor_single_scalar(tmp, ed, 1.0, op=ALU.add))
        dv(lambda: nc.vector.reciprocal(w1, tmp))
        dv(lambda: TT_(w2, ed, w1, op=ALU.mult))
        dv(lambda: TT_(w1, w1, gval, op=ALU.mult))
        dv(lambda: TT_(w2, w2, gval, op=ALU.mult))
        dv(lambda: TT_(GL, M1, bc3(w1), op=ALU.mult))
        dv(lambda: TT_(TM, M2, bc3(w2), op=ALU.mult))
        dv(lambda: TT_(GL_f, GL_f, TM_f, op=ALU.add))
        if stage == ("b0r", l):
            break
        c0 = 2 * NPL + 9
        I32v = lambda a_, n_: RS[:, a_:a_ + n_].bitcast(I32)
        POSI = I32v(3584, 16)
        POSF = rs_(3600, 16)
        NGc, NTc, BASE = rs_(3616, 4), rs_(3620, 4), rs_(3624, 4)
        GUF = rs_(3632, 7)
        CMP = rs_(3640, 16).rearrange("p (g j) -> p g j", j=4)
        CMP2 = rs_(3656, 21).rearrange("p (u g) -> p u g", g=3)
        T28 = rs_(3680, 28).rearrange("p (u j) -> p u j", j=4)
        IDX1 = I32v(3708, 28)
        IDX2 = I32v(3736, 28)
        EXC = rs_(3764, 64).rearrange("p (t g) -> p t g", g=4)
        PGF_f = rs_(3828, 64)
        PGF = PGF_f.rearrange("p (t g) -> p t g", g=4)
        GSB = bfv(rs_(3892, 32))
        GHL = bfv(rs_(3924, 128)).rearrange("p (t c) -> p t c", c=16)
        GSB3 = GSB.rearrange("p (t g) -> p t g", g=4)
        dv(lambda: nc.vector.tensor_copy(GSB, GS_f))
        ppt, ppa = PSt[2], PSB[2]
        mm(ppt, ppa[:, 0:64], UT, GSB, True, True, [rt_, UTt])
        mm(ppt, ppa[:, 64:128], ONES, GSB, True, True, [rt_, Ct])
        CNTp = ppa[:, 64:128].rearrange("p (t g) -> p t g", g=4)
        dv(lambda: nc.vector.memset(EXC[:, 0, :], 0.0))
        for tc in range(15):
            dv((lambda tc=tc: TT_(EXC[:, tc + 1, :], EXC[:, tc, :], CNTp[:, tc, :], op=ALU.add)), [ppt])
        if stage == ("s0", l):
            break
        dv((lambda: TT_(NGc, EXC[:, 15, :], CNTp[:, 15, :], op=ALU.add)), [ppt])
        if stage == ("s1", l):
            break
        dv((lambda: TT_(CMP, NGc.unsqueeze(2).to_broadcast([128, 4, 4]),
                        PV[:, c0 + 1:c0 + 5].unsqueeze(1).to_broadcast([128, 4, 4]), op=ALU.is_gt)), [PVt])
        dv(lambda: RED(out=NTc, in_=CMP, axis=AX, op=ALU.add))
        dv(lambda: nc.vector.tensor_single_scalar(NTc[:, 0:1], NTc[:, 0:1], 1.0, op=ALU.max))
        dv(lambda: nc.vector.memset(BASE[:, 0:1], 0.0))
        dv(lambda: nc.vector.tensor_copy(BASE[:, 1:2], NTc[:, 0:1]))
        dv(lambda: TT_(BASE[:, 2:3], BASE[:, 1:2], NTc[:, 1:2], op=ALU.add))
        dv(lambda: TT_(BASE[:, 3:4], BASE[:, 2:3], NTc[:, 2:3], op=ALU.add))
        if stage == ("s2", l):
            break
        dv((lambda: TT_(PGF, ppa[:, 0:64].rearrange("p (t g) -> p t g", g=4), EXC, op=ALU.add)), [ppt])
        dv(lambda: nc.vector.scalar_tensor_tensor(out=PGF, in0=BASE.unsqueeze(1).to_broadcast([128, 16, 4]), scalar=512.0,
                                                  in1=PGF, op0=ALU.mult, op1=ALU.add))
        dv(lambda: TT_(PGF_f, PGF_f, GS_f, op=ALU.mult))
        dv(lambda: RED(out=POSF, in_=PGF, axis=AX, op=ALU.add))
        if stage == ("s3", l):
            break
        dv(lambda: nc.vector.tensor_copy(POSI, POSF))
        POSH = I32v(3736, 16)
        if l > 0:
            dv(lambda l=l: nc.vector.tensor_single_scalar(POSF, POSF, float(l * NSLOT), op=ALU.add))
        dv(lambda: nc.vector.tensor_copy(POSH, POSF))
        if stage == ("b0p", l):
            break
        dv((lambda: TT_(CMP2, PV[:, c0 + 5:c0 + 12].unsqueeze(2).to_broadcast([128, 7, 3]),
                        BASE[:, 1:4].unsqueeze(1).to_broadcast([128, 7, 3]), op=ALU.is_ge)), [PVt])
        dv(lambda: RED(out=GUF, in_=CMP2, axis=AX, op=ALU.add))
        dv((lambda: nc.vector.scalar_tensor_tensor(out=T28, in0=GUF.unsqueeze(2).to_broadcast([128, 7, 4]), scalar=512.0,
                                                   in1=PV[:, c0 + 12:c0 + 16].unsqueeze(1).to_broadcast([128, 7, 4]),
                                                   op0=ALU.mult, op1=ALU.add)), [PVt])
        if l > 0:
            dv(lambda l=l: nc.vector.tensor_single_scalar(rs_(3680, 28), rs_(3680, 28), l * 2048.0, op=ALU.add))
        dv(lambda: nc.vector.tensor_copy(IDX1, T28.rearrange("p u j -> p (u j)")))
        if stage == ("b0i", l):
            break
        dv(lambda: nc.vector.memset(GHL, 0.0))
        dv(lambda: nc.vector.tensor_copy(GHL[:, :, 0:4], GL))
        dv(lambda: TT_(TM, GL, GHL[:, :, 0:4], op=ALU.subtract))
        dv(lambda: nc.vector.tensor_copy(GHL[:, :, 4:8], TM))

        HG = [bfv(RW[i]).rearrange("p (k n) -> p k n", k=8) for i in range(2)]
        HGt = [Tile("hg%d" % i, Wt[i]) for i in range(2)]
        ACTB = [bfv(RW[2 + i]).rearrange("p (k n) -> p k n", k=8) for i in range(2)]
        ACTBt = [[Tile("actb%d_%d" % (0, q), Wt[2]) for q in range(8)], None]
        YROW = [RBR[:, i * 1024:(i + 1) * 1024] for i in range(2)]
        SEL8 = bfv(RBR[0:16, 3088:3600])
        SEL8t = Tile("sel8", brbase)
        op("pool", lambda: nc.gpsimd.dma_start(out=SEL8, in_=cst[0:16, 128:128 + 1024]), writes=[SEL8t], dma="sel")
        GB = [RS[:, 2048 + i * 512:2048 + (i + 1) * 512] for i in range(2)]
        GBt = [Tile("gb%d" % i, sbase) for i in range(2)]
        GT = [bfv(RS[0:16, 3072 + i * 256:3072 + (i + 1) * 256]) for i in range(2)]
        GTt = [Tile("gt%d" % i, sbase) for i in range(2)]
        nrw = (l + 1) * NE * 128
        weg2, weu2, wed2 = wbf_g[0:nrw, :], wbf_u[0:nrw, :], wbf_d[0:nrw, :]
        IOA = bass.IndirectOffsetOnAxis
        wgu_of = {}

        def load_wgu(u, j):
            si = j
            c = u * 4 + j
            op("pool", (lambda si=si, c=c: nc.gpsimd.indirect_dma_start(
                out=WGUb[si][:, 0:2048], out_offset=None, in_=weg2, in_offset=IOA(ap=IDX1[:, c:c + 1], axis=0))),
               reads=[rt_] + PCt[l], after=[WZt], writes=[WGt[si]], dma="wg%d" % si)
            op("pool", (lambda si=si, c=c: nc.gpsimd.indirect_dma_start(
                out=WGUb[si][:, 2048:4096], out_offset=None, in_=weu2, in_offset=IOA(ap=IDX1[:, c:c + 1], axis=0))),
               reads=[rt_] + PCt[l], after=[WZt], writes=[WUt[si]], dma="wu%d" % si)
            wgu_of[(u, j)] = si

        if stage == ("b1", l):
            break
        for j in range(4):
            wgu_of[(0, j)] = j
        if stage == ("b1w", l):
            break

        rowc = [0]

        def send_rows(tc):
            i = tc % 8
            op("dve", (lambda i=i, tc=tc: nc.vector.tensor_copy(ROW[i][:, 1024:1040], GHL[:, tc, :])), reads=[rt_], writes=[ROWt[i]])
            op("pool", (lambda i=i, tc=tc, l=l: nc.gpsimd.indirect_dma_start(
                out=hs_all, out_offset=IOA(ap=POSH[:, tc:tc + 1], axis=0), in_=ROW[i], in_offset=None)),
               reads=[ROWt[i], rt_], writes=[HSt[l][i]], dma="hs%d" % i)

        for tc in range(8):
            send_rows(tc)
        for tc in range(8, 16):
            stage_rows(tc)
            send_rows(tc)
        WDUf = [bfv(RH[:, i * 4096:(i + 1) * 4096]) for i in range(2)]
        WDU = [w_.rearrange("p (q n) -> p q n", q=8) for w_ in WDUf]
        hdead = merged_base(flat(Ht))
        WDt = [[Tile("wd%d_%d" % (i, j), hdead) for j in range(4)] for i in range(2)]

        def load_wd(u, j):
            ub = u % 2
            c = u * 4 + j
            op("pool", (lambda ub=ub, j=j, c=c: nc.gpsimd.indirect_dma_start(
                out=WDUf[ub][:, 2 * j * 1024:(2 * j + 2) * 1024], out_offset=None, in_=wed2,
                in_offset=IOA(ap=IDX1[:, c:c + 1], axis=0))),
               reads=[rt_] + PCt[l], after=[WZt], writes=[WDt[ub][j]], dma="wd%d_%d" % (ub, j))

        if stage == ("b2s", l):
            break
        for j in range(4):
            load_wd(0, j)
        if stage == ("b2", l):
            break

        def CU(u, part=None):
            ub = u % 2
            pgt, pga = ps()
            pgb = pga.bitcast(BF16)
            s4s = list(range(4) if part is None else range(2 * part, 2 * part + 2))
            for s4 in s4s:
                i = (rowc[0] % 2) if u > 0 else (2 + s4)
                rowc[0] += 1
                r0 = u * 512 + s4 * 128
                op("sp", (lambda i=i, r0=r0, l=l: nc.sync.dma_start(out=ROW[i], in_=hs_all[l * NSLOT + r0:l * NSLOT + r0 + 128, :])),
                   reads=HSt[l], writes=[ROWt[i]], dma="hl%d" % i)
                pt, pa = ps()
                pbf = pa.bitcast(BF16)
                for k in range(8):
                    op("pe", (lambda pbf=pbf, k=k, i=i: nc.tensor.transpose(pbf[:, k * 128:(k + 1) * 128], ROWK[i][:, k, :], IDENT)),
                       reads=[ROWt[i], Ct], writes=[pt])
                op("pe", (lambda pgb=pgb, s4=s4, i=i: nc.tensor.transpose(pgb[0:16, s4 * 128:(s4 + 1) * 128], ROW[i][:, 1024:1040], IDENT)),
                   reads=[ROWt[i], Ct], writes=[pgt])
                op("act", (lambda ub=ub, s4=s4, pbf=pbf: nc.scalar.activation(
                    HG[ub][:, :, s4 * 128:(s4 + 1) * 128], pbf.rearrange("p (k n) -> p k n", k=8), AF.Copy)),
                   reads=[pt], writes=[HGt[ub]])
            c_lo, c_hi = s4s[0] * 128, (s4s[-1] + 1) * 128
            op("act", (lambda ub=ub, pgb=pgb, c_lo=c_lo, c_hi=c_hi: nc.scalar.activation(
                GT[ub][:, c_lo:c_hi], pgb[0:16, c_lo:c_hi], AF.Copy)), reads=[pgt], writes=[GTt[ub]])

        gbc = [0]

        def GUS(u, j):
            ub = u % 2
            si = wgu_of[(u, j)]
            WG = WGUb[si][:, 0:2048].rearrange("p (k n) -> p k n", k=8)
            WU = WGUb[si][:, 2048:4096].rearrange("p (k n) -> p k n", k=8)
            pGt, pGa = ps()
            mm(pGt, pGa, SEL8[:, j * 128:(j + 1) * 128], GT[ub], True, False, [SEL8t, GTt[ub]])
            mm(pGt, pGa, SEL8[:, (4 + j) * 128:(5 + j) * 128], GT[ub], False, True, [SEL8t, GTt[ub]])
            gi = gbc[0] % 2
            gbc[0] += 1
            op("act", (lambda gi=gi, pGa=pGa: nc.scalar.activation(GB[gi], pGa, AF.Copy)), reads=[pGt], writes=[GBt[gi]])
            for f in range(2):
                pgt, pga = ps()
                for k in range(8):
                    mm(pgt, pga, WG[:, k, f * 128:(f + 1) * 128], HG[ub][:, k, :], k == 0, k == 7, [WGt[si], HGt[ub]])
                put, pua = ps()
                for k in range(8):
                    mm(put, pua, WU[:, k, f * 128:(f + 1) * 128], HG[ub][:, k, :], k == 0, k == 7, [WUt[si], HGt[ub]])
                jj = nxt("sg")
                op("act", (lambda jj=jj, pga=pga: nc.scalar.activation(SG[jj], pga, AF.Silu)), reads=[pgt], writes=[SGt[jj]])
                op("dve", (lambda jj=jj, pua=pua: nc.vector.tensor_tensor(SG[jj], SG[jj], pua, op=ALU.mult)),
                   reads=[put, SGt[jj]], writes=[SGt[jj]])
                q = 2 * j + f
                op("dve", (lambda jj=jj, gi=gi, ub=ub, q=q: nc.vector.tensor_tensor(ACTB[ub][:, q, :], SG[jj], GB[gi], op=ALU.mult)),
                   reads=[GBt[gi], SGt[jj]], writes=[ACTBt[ub][q]])

        yc = [0]

        def DOWNS(u):
            ub = u % 2
            for s4 in range(4):
                yi = yc[0] % 2
                yc[0] += 1
                for half in range(2):
                    pt, pa = ps()
                    for q in range(8):
                        mm(pt, pa, ACTB[ub][:, q, s4 * 128:(s4 + 1) * 128], WDU[ub][:, q, half * 512:(half + 1) * 512],
                           q == 0, q == 7, [ACTBt[ub][q], WDt[ub][q // 2]])
                    dst = YROW[yi][:, half * 512:(half + 1) * 512]
                    if half == 0:
                        op("act", (lambda dst=dst, pa=pa: nc.scalar.activation(dst, pa, AF.Copy)), reads=[pt], writes=[YROWt[yi]])
                    else:
                        op("dve", (lambda dst=dst, pa=pa: nc.vector.tensor_copy(dst, pa)), reads=[pt], writes=[YROWt[yi]])
                r0 = u * 512 + s4 * 128
                op("sp", (lambda yi=yi, r0=r0: nc.sync.dma_start(out=ys_d[r0:r0 + 128, :], in_=YROW[yi])),
                   reads=[YROWt[yi]], writes=[YSt[yi]], dma="ys%d" % yi)

        last_layer = (l == (layers[-1] if layers is not None else n_layers - 1))
        fuse_next = (stage is None)
        Hnx = [[None] * NT for _ in range(8)]
        CU(0)
        YROWt = [Tile("yrow%d" % i, merged_base(ROWt[2:5])) for i in range(2)]
        ACTBt[1] = [Tile("actb%d_%d" % (1, q), merged_base(ROWt[5:8])) for q in range(8)]
        for u in range(NU):
            for j in range(4):
                if j == 3 and u + 1 < NU:
                    CU(u + 1, 0)
                GUS(u, j)
                if u + 1 < NU:
                    load_wgu(u + 1, j)
            if u + 1 < NU:
                CU(u + 1, 1)
            if u >= 1:
                DOWNS(u - 1)
            if u + 1 < NU:
                for j in range(4):
                    load_wd(u + 1, j)
        DOWNS(NU - 1)
        if stage == ("b3", l):
            break
        preloaded = False
        if fuse_next and not last_layer:
            for i_, tl_ in ((3, ACTBt[1]), (2, ACTBt[0])):
                mb_ = merged_base(tl_)
                Wt[i_].w = mb_.w
                Wt[i_].r = {}
            pre_w[0] = (load_kn(w_in[l + 1], 0, 512, slot=3), load_kn(w_in[l + 1], 512, 512, slot=2))
            wctr[0] = 0
            preloaded = True

        IDF = RS[:, 3072:3200]
        IDFt = Tile("idf", merged_base(GTt))
        op("sp", lambda: nc.sync.dma_start(out=IDF, in_=cst[:, 0:128]), writes=[IDFt], dma="idf")
        YT = ([RW[1][:, i * 1024:(i + 1) * 1024] for i in range(2)] + [RW[0][:, i * 1024:(i + 1) * 1024] for i in range(2)]
              + [RS[:, 2048:3072]])
        YTt = ([Tile("yt%d" % i, HGt[1]) for i in range(2)] + [Tile("yt%d" % (2 + i), HGt[0]) for i in range(2)]
               + [Tile("yt4", merged_base(GBt))])
        obbase[0] = merged_base([rt_])
        hb2 = merged_base(flat(WDt))
        ygc = [0]
        for tt in range(NT):
            banks = [ps() for _ in range(8)]
            for tci in range(4):
                tc = tt * 4 + tci
                b_ = ygc[0] % 5
                ygc[0] += 1
                op("pool", (lambda b_=b_, tc=tc: nc.gpsimd.indirect_dma_start(
                    out=YT[b_], out_offset=None, in_=ys_d, in_offset=IOA(ap=POSI[:, tc:tc + 1], axis=0))),
                   reads=YSt + [rt_], writes=[YTt[b_]], dma="yg%d" % b_)
                for oc in range(8):
                    pt, pa = banks[oc]
                    op("pe", (lambda pa=pa, tci=tci, b_=b_, oc=oc: nc.tensor.transpose(
                        pa[:, tci * 128:(tci + 1) * 128], YT[b_][:, oc * 128:(oc + 1) * 128], IDF)),
                       reads=[YTt[b_], IDFt], writes=[pt])
            for oc in range(8):
                pt, pa = banks[oc]
                dst = X[oc][:, tsl(tt)]
                op("dve", (lambda dst=dst, pa=pa: nc.vector.tensor_tensor(dst, dst, pa, op=ALU.add)),
                   reads=[pt, Xt[oc][tt]], writes=[Xt[oc][tt]])
            if fuse_next:
                if not last_layer:
                    for c in range(8):
                        Hnx[c][tt] = Tile("h%d_%d" % (c, tt), hb2)
                    norm_tile(tt, (l + 1) * NPL, Hnx)
                else:
                    final_tile(tt)
        mgbase = merged_base(WGt + WUt)
        sbase = merged_base([rt_] + GBt + GTt + [IDFt, YTt[4]])
        brbase = merged_base(YROWt + ROWt[0:5] + [SEL8t])
        hbase = hb2
        for i_, tl_ in ((0, [HGt[0]] + YTt[2:4]), (1, [HGt[1]] + YTt[0:2]), (2, ACTBt[0]), (3, ACTBt[1])):
            if preloaded and i_ >= 2:
                continue
            mb_ = merged_base(tl_)
            Wt[i_].w = mb_.w
            Wt[i_].r = {}
        Ht_pre = Hnx if (fuse_next and not last_layer) else None
        if stage == ("moe", l):
            break

    if stage is not None:
        for c in range(8):
            ev = op("sp", (lambda c=c: nc.sync.dma_start(out=outT[c * 128:(c + 1) * 128, :], in_=X[c])),
                    reads=Xt[c], dma="o%d" % (c % 4))
            final.append(ev)
    last = {}
    for k, v in final:
        last[k] = max(last.get(k, 0), v)
    P.emit(list(last.items()))
    return nc, P


def _cols(v):
    return np.ascontiguousarray(np.asarray(v, np.float32).reshape(-1, 128).T)


def make_shared(inp):
    g = lambda k: np.asarray(inp[k], np.float32)
    pvn = np.zeros((128, NPV), np.float32)
    for l in range(L):
        b = l * NPL
        pvn[:, b + 0:b + 8] = _cols(g("norm_mix_g")[l])
        pvn[:, b + 8:b + 16] = _cols(g("norm_x_g")[l])
        pvn[:, b + 16:b + 24] = _cols(g("norm_ffn_g")[l])
        pvn[:, b + 24:b + 32] = _cols(g("norm_mem_g")[l])
        pvn[:, b + 32:b + 40] = _cols(g("pool_scale")[l])
        caw = g("conv_a_w")[l]
        for c in range(4):
            pvn[:, b + 40 + c * 31:b + 40 + (c + 1) * 31] = caw[:, c * 128:(c + 1) * 128].T
        pvn[:, b + 164:b + 168] = _cols(g("conv_a_b")[l])
        pvn[:, b + 168:b + 172] = _cols(g("ln_a_g")[l])
        pvn[:, b + 172:b + 176] = _cols(g("ln_a_b")[l])
        ccw = g("conv_c_w")[l]
        for c in range(4):
            pvn[:, b + 176 + c * 3:b + 176 + (c + 1) * 3] = ccw[:, c * 128:(c + 1) * 128].T
        pvn[:, b + 188:b + 192] = g("b_rg")[l][None, :]
        pvn[:, b + 192:b + 208] = g("b_re")[l][None, :]
    pvn[:, 2 * NPL:2 * NPL + 8] = _cols(g("norm_f_g"))
    pvn[:, 2 * NPL + 8] = EPS
    c0 = 2 * NPL + 9
    pidx = np.arange(128, dtype=np.float32)
    pvn[:, c0] = pidx
    pvn[:, c0 + 1:c0 + 5] = np.array([0.0, 512.0, 1024.0, 1536.0], np.float32)[None, :]
    pvn[:, c0 + 5:c0 + 12] = np.arange(7, dtype=np.float32)[None, :]
    for j in range(4):
        pvn[:, c0 + 12 + j] = j * 128 + pidx
        pvn[:, c0 + 16 + j] = j * 256 + pidx
    cst = np.zeros((128, 128 + 2048 + 128), np.float32)
    cst[:, 128 + 2048:] = (pidx[:, None] < pidx[None, :]).astype(np.float32)
    cst[:, 0:128] = np.eye(128, dtype=np.float32)
    for e in range(16):
        cst[e, 128 + e * 128:128 + (e + 1) * 128] = 1.0
    shared = {
        "pv": pvn, "cst": cst,
        "w_in": g("w_in"), "w_a_out": g("w_a_out"), "w_pool": g("w_pool_grp"), "w_c_out": g("w_c_out"),
        "w_o": g("w_o"), "w_xq": g("w_xq"), "w_xkv": g("w_xkv"), "w_xo": g("w_xo"),
        "w_r": np.ascontiguousarray(np.concatenate([g("w_rg"), g("w_re")], axis=-1)),
    }
    shared["w_eg"] = g("w_e_gate")
    shared["w_eu"] = g("w_e_up")
    shared["w_ed"] = np.ascontiguousarray(
        g("w_e_down").reshape(L, NE, 2, 128, D).transpose(0, 1, 3, 2, 4)).reshape(L, NE, 128, 2 * D)
    return shared


_CACHE = {}


def kernel(**inputs):
    x = np.asarray(inputs["x"], np.float32)
    mem = np.asarray(inputs["mem"], np.float32)
    shared = make_shared(inputs)
    if "nc" not in _CACHE:
        _CACHE["nc"] = build()[0]
    nc = _CACHE["nc"]
    B = x.shape[0]
    in_maps = []
    for b in range(B):
        m = dict(shared)
        m["xT"] = np.ascontiguousarray(x[b].T)
        m["memT"] = np.ascontiguousarray(mem[b].T)
        in_maps.append(m)
    res = run_bass_kernel_spmd(nc, in_maps, core_ids=list(range(B)))
    out = np.stack([np.ascontiguousarray(res.results[b]["outT"].T) for b in range(B)], axis=0)
    return out.astype(np.float32)
```
